# Optimizing a Trainium2 kernel written in Bass

```python
import jax, jax.numpy as jnp
from jax import lax
import numpy as np

D_MODEL = 1024
BATCH = 32
SEQ = 2048
DEPTH = 1

PLE_DIM = 256
EPS = 1e-6

SSD_HEADDIM = 64
SSD_INNER = D_MODEL
SSD_HEADS = SSD_INNER // SSD_HEADDIM
SSD_GROUPS = 2
SSD_STATE = 128
SSD_CONV = 4
SSD_CHUNK = 128
SSD_CONV_CH = SSD_INNER + 2 * SSD_GROUPS * SSD_STATE

CONF_CH = D_MODEL
CONF_KERNEL = 31

OFF_XBC = SSD_INNER
OFF_DT = OFF_XBC + SSD_CONV_CH
OFF_GLU = OFF_DT + SSD_HEADS
OFF_GATE = OFF_GLU + 2 * CONF_CH
N_IN = OFF_GATE + 2 * D_MODEL

N_GROUPS = 4
EXPERTS_PER_GROUP = 8
N_EXPERTS = N_GROUPS * EXPERTS_PER_GROUP
TOP_K = 2
D_EXPERT = 512
MOE_BLOCK = 256

kernel_name = 'hybrid_ssd_conformer_hmoe_block'


def rms_norm(x, g):
    xf = x.astype(jnp.float32)
    y = xf * lax.rsqrt(jnp.mean(xf * xf, axis=-1, keepdims=True) + EPS)
    return (y * g.astype(jnp.float32)).astype(x.dtype)


def layer_norm(x, g, b):
    xf = x.astype(jnp.float32)
    mu = jnp.mean(xf, axis=-1, keepdims=True)
    xc = xf - mu
    y = xc * lax.rsqrt(jnp.mean(xc * xc, axis=-1, keepdims=True) + EPS)
    return (y * g.astype(jnp.float32) + b.astype(jnp.float32)).astype(x.dtype)


def causal_dwconv(x, w, b):
    k, c = w.shape
    y = lax.conv_general_dilated(
        x, w[:, None, :].astype(x.dtype), window_strides=(1,), padding=((k - 1, 0),),
        dimension_numbers=('NWC', 'WIO', 'NWC'), feature_group_count=c)
    return y + b.astype(x.dtype)


def ssd_chunked(x, dt, a, b_mat, c_mat):
    bsz, seq, h, p = x.shape
    g, n = b_mat.shape[2], b_mat.shape[3]
    r = h // g
    q = SSD_CHUNK
    nc = seq // q
    xdt = (x * dt[..., None]).reshape(bsz, nc, q, g, r, p)
    a_cs = jnp.cumsum((dt * a).reshape(bsz, nc, q, g, r), axis=2)
    bc = b_mat.reshape(bsz, nc, q, g, n)
    cc = c_mat.reshape(bsz, nc, q, g, n)
    causal = jnp.tril(jnp.ones((q, q), dtype=bool))[None, None, :, :, None, None]
    seg = a_cs[:, :, :, None] - a_cs[:, :, None, :]
    decay = jnp.exp(jnp.where(causal, seg, -jnp.inf))
    cb = jnp.einsum('bclgn,bcsgn->bclsg', cc, bc)
    y_diag = jnp.einsum('bclsgr,bcsgrp->bclgrp', cb[..., None] * decay, xdt)
    decay_end = jnp.exp(a_cs[:, :, -1:] - a_cs)
    states = jnp.einsum('bcsgn,bcsgrp->bcgrpn', bc, xdt * decay_end[..., None])
    chunk_decay = jnp.exp(a_cs[:, :, -1])

    def step(carry, inp):
        st, dec = inp
        return carry * dec[..., None, None] + st, carry

    init = jnp.zeros((bsz, g, r, p, n), jnp.float32)
    _, prev = lax.scan(step, init, (jnp.moveaxis(states, 1, 0), jnp.moveaxis(chunk_decay, 1, 0)))
    prev = jnp.moveaxis(prev, 0, 1)
    y_off = jnp.einsum('bclgn,bcgrpn->bclgrp', cc, prev) * jnp.exp(a_cs)[..., None]
    return (y_diag + y_off).reshape(bsz, seq, h, p)


def ssd_branch(proj, conv_w, conv_b, dt_bias, a_log, d_skip, norm_g, w_out):
    bsz, seq, _ = proj.shape
    z = proj[..., :SSD_INNER]
    xbc = jax.nn.silu(causal_dwconv(proj[..., OFF_XBC:OFF_DT], conv_w, conv_b))
    dt_raw = proj[..., OFF_DT:OFF_GLU]
    gn = SSD_GROUPS * SSD_STATE
    xs = xbc[..., :SSD_INNER].astype(jnp.float32).reshape(bsz, seq, SSD_HEADS, SSD_HEADDIM)
    bm = xbc[..., SSD_INNER:SSD_INNER + gn].astype(jnp.float32).reshape(bsz, seq, SSD_GROUPS, SSD_STATE)
    cm = xbc[..., SSD_INNER + gn:].astype(jnp.float32).reshape(bsz, seq, SSD_GROUPS, SSD_STATE)
    dt = jax.nn.softplus(dt_raw.astype(jnp.float32) + dt_bias.astype(jnp.float32))
    a = -jnp.exp(a_log.astype(jnp.float32))
    y = ssd_chunked(xs, dt, a, bm, cm) + d_skip.astype(jnp.float32)[:, None] * xs
    y = y.reshape(bsz, seq, SSD_INNER).astype(proj.dtype)
    y = rms_norm(y * jax.nn.silu(z), norm_g)
    return y @ w_out


def conformer_branch(proj, dw_w, dw_b, ln_g, ln_b, w_out):
    glu = proj[..., OFF_GLU:OFF_GLU + CONF_CH] * jax.nn.sigmoid(proj[..., OFF_GLU + CONF_CH:OFF_GATE])
    c = causal_dwconv(glu, dw_w, dw_b)
    c = jax.nn.silu(layer_norm(c, ln_g, ln_b))
    return c @ w_out


def hier_moe(u, wg, bg, we, be, w_gate, w_up, w_down):
    n, d = u.shape
    g_logits = (u @ wg).astype(jnp.float32) + bg.astype(jnp.float32)
    g_idx = jnp.argmax(g_logits, axis=-1)
    g_prob = jnp.take_along_axis(jax.nn.softmax(g_logits, axis=-1), g_idx[:, None], axis=-1)
    e_logits = ((u @ we).astype(jnp.float32) + be.astype(jnp.float32)).reshape(n, N_GROUPS, EXPERTS_PER_GROUP)
    e_in = jnp.take_along_axis(e_logits, g_idx[:, None, None], axis=1)[:, 0]
    top_v, top_i = lax.top_k(e_in, TOP_K)
    gate_w = jax.nn.softmax(top_v, axis=-1) * g_prob
    expert = g_idx[:, None] * EXPERTS_PER_GROUP + top_i
    n_assign = n * TOP_K
    flat_e = expert.reshape(n_assign)
    order = jnp.argsort(flat_e)
    sorted_e = flat_e[order]
    sorted_tok = order // TOP_K
    sorted_w = gate_w.reshape(n_assign)[order].astype(u.dtype)
    counts = jnp.bincount(flat_e, length=N_EXPERTS)
    padded = (counts + MOE_BLOCK - 1) // MOE_BLOCK * MOE_BLOCK
    pad_end = jnp.cumsum(padded)
    pad_start = pad_end - padded
    start = jnp.cumsum(counts) - counts
    dest = pad_start[sorted_e] + jnp.arange(n_assign) - start[sorted_e]
    n_blocks = -(-n_assign // MOE_BLOCK) + N_EXPERTS
    rows = n_blocks * MOE_BLOCK
    buf = jnp.zeros((rows, d), u.dtype).at[dest].set(u[sorted_tok])
    blk_e = jnp.minimum(jnp.searchsorted(pad_end, jnp.arange(n_blocks) * MOE_BLOCK, side='right'), N_EXPERTS - 1)

    def expert_block(args):
        xb, e = args
        hdn = jax.nn.silu(xb @ w_gate[e]) * (xb @ w_up[e])
        return hdn @ w_down[e]

    yb = lax.map(expert_block, (buf.reshape(n_blocks, MOE_BLOCK, d), blk_e)).reshape(rows, d)
    contrib = yb[dest] * sorted_w[:, None]
    return jnp.zeros((n, d), u.dtype).at[sorted_tok].add(contrib)


def setup_inputs(seed: int = 0) -> dict:
    key = jax.random.key(seed)
    ks = iter(jax.random.split(key, 40))
    f32 = jnp.float32

    def nrm(shape, scale):
        return jax.random.normal(next(ks), shape, f32) * scale

    L = DEPTH
    dt0 = jnp.exp(jax.random.uniform(next(ks), (L, SSD_HEADS), f32, np.log(1e-3), np.log(1e-1)))
    dt_bias = dt0 + jnp.log(-jnp.expm1(-dt0))
    a_log = jnp.log(jax.random.uniform(next(ks), (L, SSD_HEADS), f32, 1.0, 16.0))
    return {
        'x': nrm((BATCH, SEQ, D_MODEL), 1.0),
        'p': nrm((DEPTH, BATCH, SEQ, PLE_DIM), 1.0),
        'norm_mix_g': 1.0 + nrm((L, D_MODEL), 0.01),
        'w_in': nrm((L, D_MODEL, N_IN), D_MODEL ** -0.5),
        'ssd_conv_w': nrm((L, SSD_CONV, SSD_CONV_CH), SSD_CONV ** -0.5),
        'ssd_conv_b': nrm((L, SSD_CONV_CH), 0.01),
        'ssd_dt_bias': dt_bias,
        'ssd_a_log': a_log,
        'ssd_d': 1.0 + nrm((L, SSD_HEADS), 0.1),
        'ssd_norm_g': 1.0 + nrm((L, SSD_INNER), 0.01),
        'w_ssd_out': nrm((L, SSD_INNER, D_MODEL), SSD_INNER ** -0.5),
        'conf_dw_w': nrm((L, CONF_KERNEL, CONF_CH), CONF_KERNEL ** -0.5),
        'conf_dw_b': nrm((L, CONF_CH), 0.01),
        'conf_ln_g': 1.0 + nrm((L, CONF_CH), 0.01),
        'conf_ln_b': nrm((L, CONF_CH), 0.01),
        'w_conf_out': nrm((L, CONF_CH, D_MODEL), CONF_CH ** -0.5),
        'w_o': nrm((L, D_MODEL, D_MODEL), D_MODEL ** -0.5),
        'norm_ffn_g': 1.0 + nrm((L, D_MODEL), 0.01),
        'router_group_w': nrm((L, D_MODEL, N_GROUPS), D_MODEL ** -0.5),
        'router_group_b': nrm((L, N_GROUPS), 0.01),
        'router_expert_w': nrm((L, D_MODEL, N_EXPERTS), D_MODEL ** -0.5),
        'router_expert_b': nrm((L, N_EXPERTS), 0.01),
        'expert_w_gate': nrm((L, N_EXPERTS, D_MODEL, D_EXPERT), D_MODEL ** -0.5),
        'expert_w_up': nrm((L, N_EXPERTS, D_MODEL, D_EXPERT), D_MODEL ** -0.5),
        'expert_w_down': nrm((L, N_EXPERTS, D_EXPERT, D_MODEL), D_EXPERT ** -0.5),
        'norm_ple_g': 1.0 + nrm((L, D_MODEL), 0.01),
        'w_ple_gate': nrm((L, D_MODEL, D_MODEL), D_MODEL ** -0.5),
        'w_ple_proj': nrm((L, PLE_DIM, D_MODEL), PLE_DIM ** -0.5),
        'final_norm_g': 1.0 + nrm((D_MODEL,), 0.01),
    }


def reference(x, p, norm_mix_g, w_in, ssd_conv_w, ssd_conv_b, ssd_dt_bias, ssd_a_log, ssd_d,
              ssd_norm_g, w_ssd_out, conf_dw_w, conf_dw_b, conf_ln_g, conf_ln_b, w_conf_out, w_o,
              norm_ffn_g, router_group_w, router_group_b, router_expert_w, router_expert_b,
              expert_w_gate, expert_w_up, expert_w_down, norm_ple_g, w_ple_gate, w_ple_proj,
              final_norm_g):
    bsz, seq, d = x.shape
    h = x
    for i in range(DEPTH):
        u = rms_norm(h, norm_mix_g[i])
        proj = u @ w_in[i]
        y_ssd = ssd_branch(proj, ssd_conv_w[i], ssd_conv_b[i], ssd_dt_bias[i], ssd_a_log[i],
                           ssd_d[i], ssd_norm_g[i], w_ssd_out[i])
        y_conv = conformer_branch(proj, conf_dw_w[i], conf_dw_b[i], conf_ln_g[i], conf_ln_b[i],
                                  w_conf_out[i])
        gate_ssd = jax.nn.sigmoid(proj[..., OFF_GATE:OFF_GATE + D_MODEL])
        gate_conv = jax.nn.sigmoid(proj[..., OFF_GATE + D_MODEL:])
        h = h + (gate_ssd * y_ssd + gate_conv * y_conv) @ w_o[i]
        u = rms_norm(h, norm_ffn_g[i]).reshape(bsz * seq, d)
        moe = hier_moe(u, router_group_w[i], router_group_b[i], router_expert_w[i],
                       router_expert_b[i], expert_w_gate[i], expert_w_up[i], expert_w_down[i])
        h = h + moe.reshape(bsz, seq, d)
        ple_gate = jax.nn.sigmoid(rms_norm(h, norm_ple_g[i]) @ w_ple_gate[i])
        h = h + (p[i] @ w_ple_proj[i]) * ple_gate
    return rms_norm(h, final_norm_g)
```

```python
import contextlib
import numpy as np
import concourse.bass as bass
import concourse.mybir as mybir
from concourse.bass_utils import run_bass_kernel_spmd

F32 = mybir.dt.float32
BF16 = mybir.dt.bfloat16
I32 = mybir.dt.int32
AF = mybir.ActivationFunctionType
ALU = mybir.AluOpType
AX = mybir.AxisListType

D = 1024
SEQ = 2048
NIN = 6672
EPS = 1e-6
OFF_XBC, OFF_DT, OFF_GLU, OFF_GATE = 1024, 2560, 2576, 4624
NEXP = 32
DEXP = 512
MB = 512
ROT = 30000
B_OFFSET = 7
C1_OFFSET = 5


class Buf:
    _n = 0

    def __init__(self, name, t=None, sem=None):
        Buf._n += 1
        self.uid = Buf._n
        self.name = name
        self.t = t
        self.lw = None
        self.rd = {}
        self.sem = sem
        self.dn = 0
        self.dw = 0

    def __getitem__(self, k):
        return self.t[k]


class Sched:
    CE = ("pe", "act", "dve", "pool")

    def __init__(self, nc, es):
        self.nc = nc
        self.es = es
        self.e = {"pe": nc.tensor, "act": nc.scalar, "dve": nc.vector, "pool": nc.gpsimd, "sp": nc.sync}
        self.cnt = {k: 0 for k in self.CE}
        self.sems = {k: [] for k in self.CE}
        self.waited = {}
        self.dbufs = []
        self.nsem = 0
        self.sempool = []

    def newsem(self, name):
        self.nsem += 1
        return self.es.enter_context(self.nc.semaphore(f"{name}_{self.nsem}"))

    def buf(self, name, t=None, dma=False):
        b = Buf(name, t, None)
        if dma:
            if self.sempool:
                b.sem, b.dn = self.sempool.pop()
            else:
                b.sem = self.newsem("dma")
            self.dbufs.append(b)
        return b

    def release(self, bufs):
        for b in bufs:
            if b.sem is not None and b in self.dbufs:
                self.dbufs.remove(b)
                self.sempool.append((b.sem, b.dn))
                b.sem = None

    def _semval(self, eng, seq):
        i = (seq - 1) // ROT
        while len(self.sems[eng]) <= i:
            self.sems[eng].append(self.newsem(eng))
        return self.sems[eng][i], (seq - 1) % ROT + 1

    def _emit_waits(self, waiter, cdeps, ddeps):
        w = self.e[waiter]
        for src, seq in cdeps.items():
            if waiter == "pe" and src == "pe":
                continue
            key = (waiter, src)
            if self.waited.get(key, 0) >= seq:
                continue
            self.waited[key] = seq
            sem, val = self._semval(src, seq)
            w.wait_ge(sem, val)
        for b, n in ddeps.items():
            key = (waiter, "d", b.uid)
            if self.waited.get(key, 0) >= n:
                continue
            self.waited[key] = n
            w.wait_ge(b.sem, 16 * n)

    @staticmethod
    def _deps(r, w):
        cd, dd = {}, {}

        def addc(x):
            if x is not None and cd.get(x[0], 0) < x[1]:
                cd[x[0]] = x[1]

        for b in r:
            addc(b.lw)
            if b.dw:
                dd[b] = max(dd.get(b, 0), b.dw)
        for b in w:
            addc(b.lw)
            for e_, s_ in b.rd.items():
                addc((e_, s_))
            if b.dn:
                dd[b] = max(dd.get(b, 0), b.dn)
        return cd, dd

    def op(self, eng, fn, r=(), w=()):
        cd, dd = self._deps(r, w)
        self._emit_waits(eng, cd, dd)
        ins = fn(self.e[eng])
        self.cnt[eng] += 1
        seq = self.cnt[eng]
        sem, _ = self._semval(eng, seq)
        ins.then_inc(sem, 1)
        for b in r:
            b.rd[eng] = seq
        for b in w:
            b.lw = (eng, seq)
            b.rd = {}

    def dma(self, q, fn, prim, r=(), w=()):
        cd, dd = self._deps(r, w)
        self._emit_waits(q, cd, dd)
        ins = fn(self.e[q])
        ins.then_inc(prim.sem, 16)
        prim.dn += 1
        if prim in w:
            prim.dw = prim.dn

    def barrier(self):
        for waiter in ("pe", "act", "dve", "pool", "sp"):
            cd = {k: self.cnt[k] for k in self.CE if self.cnt[k] > 0}
            dd = {b: b.dn for b in self.dbufs if b.dn > 0}
            w = self.e[waiter]
            for src, seq in cd.items():
                key = (waiter, src)
                if self.waited.get(key, 0) >= seq:
                    continue
                self.waited[key] = seq
                sem, val = self._semval(src, seq)
                w.wait_ge(sem, val)
            for b, n in dd.items():
                key = (waiter, "d", b.uid)
                if self.waited.get(key, 0) >= n:
                    continue
                self.waited[key] = n
                w.wait_ge(b.sem, 16 * n)


class Ctx:
    pass


def _mm(out, lhsT, rhs, start, stop, skip=False):
    if skip:
        return lambda e: e.matmul(out, lhsT, rhs, start=start, stop=stop, skip_group_check=True)
    return lambda e: e.matmul(out, lhsT, rhs, start=start, stop=stop)


def build(nseq, debug=False, upto="E"):
    T = nseq * SEQ
    NT = T // 128
    NG = T // 512
    NBLK = (2 * T) // MB + NEXP
    NROWS = NBLK * MB
    nc = bass.Bass("TRN2", target_bir_lowering=False)
    K = Ctx()
    K.nc, K.T, K.NT, K.NG, K.NBLK, K.NROWS, K.nseq = nc, T, NT, NG, NBLK, NROWS, nseq

    def din(name, shape, dt=F32):
        return nc.dram_tensor(name, list(shape), dt, kind="ExternalInput").ap()

    def dscr(name, shape, dt):
        return nc.dram_tensor(name, list(shape), dt, kind="ExternalOutput" if debug else "Internal").ap()

    I = {}
    I["x"] = din("x", [T, D])
    I["p"] = din("p", [T, 256])
    I["norm_mix_g"] = din("norm_mix_g", [D])
    I["w_in"] = din("w_in", [D, NIN])
    I["ssd_conv_w"] = din("ssd_conv_w", [4, 1536])
    I["ssd_conv_b"] = din("ssd_conv_b", [1536])
    I["ssd_dt_bias"] = din("ssd_dt_bias", [16])
    I["ssd_a_log"] = din("ssd_a_log", [16])
    I["ssd_d"] = din("ssd_d", [16])
    I["ssd_norm_g"] = din("ssd_norm_g", [D])
    I["w_ssd_out"] = din("w_ssd_out", [D, D])
    I["conf_dw_w"] = din("conf_dw_w", [31, D])
    I["conf_dw_b"] = din("conf_dw_b", [D])
    I["conf_ln_g"] = din("conf_ln_g", [D])
    I["conf_ln_b"] = din("conf_ln_b", [D])
    I["w_conf_out"] = din("w_conf_out", [D, D])
    I["w_o"] = din("w_o", [D, D])
    I["norm_ffn_g"] = din("norm_ffn_g", [D])
    I["router_w"] = din("router_w", [D, 36])
    I["router_b"] = din("router_b", [36])
    I["expert_w_gate"] = din("expert_w_gate", [NEXP * D * DEXP // 2048, 2048])
    I["expert_w_up"] = din("expert_w_up", [NEXP * D * DEXP // 2048, 2048])
    I["expert_w_down"] = din("expert_w_down", [NEXP * DEXP, D])
    I["norm_ple_g"] = din("norm_ple_g", [D])
    I["w_ple_gate"] = din("w_ple_gate", [D, D])
    I["w_ple_proj"] = din("w_ple_proj", [256, D])
    I["final_norm_g"] = din("final_norm_g", [D])
    I["c_ident"] = din("c_ident", [128, 128])
    I["c_trile"] = din("c_trile", [128, 128])
    I["c_ustr"] = din("c_ustr", [128, 128])
    I["c_e3"] = din("c_e3", [16, 16 * 128])
    I["c_iota"] = din("c_iota", [128, 1])
    I["c_bpos"] = din("c_bpos", [128, NBLK])
    K.I = I
    K.out = nc.dram_tensor("out", [T, D], F32, kind="ExternalOutput").ap()
    Dd = {}
    Dd["SZ"] = dscr("SZ", [T, D], BF16)
    Dd["XBCT"] = dscr("XBCT", [1536, T], BF16)
    Dd["GLUT"] = dscr("GLUT", [D, T], BF16)
    Dd["GATET"] = dscr("GATET", [2 * D, T], BF16)
    Dd["DTR"] = dscr("DTR", [T, 16], F32)
    Dd["GS1T"] = dscr("GS1T", [D, T], BF16)
    Dd["GST"] = dscr("GST", [D, T], BF16)
    Dd["H1"] = dscr("H1", [T, D], F32)
    Dd["U2"] = dscr("U2", [T, D], BF16)
    Dd["XS"] = dscr("XS", [NROWS, D], BF16)
    Dd["Y"] = dscr("Y", [NROWS, D], BF16)
    K.Dd = Dd
    K.debug = debug

    with contextlib.ExitStack() as es:
        S = Sched(nc, es)
        K.S = S
        with contextlib.ExitStack() as pes:
            phase_A(K, pes)
            S.barrier()
        if upto >= "B":
            with contextlib.ExitStack() as pes:
                phase_B(K, pes)
                S.barrier()
        if upto >= "C":
            with contextlib.ExitStack() as pes2:
                phase_CDE(K, pes2, upto)
                S.barrier()
    return nc


def mk_alloc(K, pes):
    nc, S = K.nc, K.S

    K.uid = getattr(K, "uid", 0) + 1
    pfx = f"P{K.uid}_"
    mine = []
    pes.callback(lambda: S.release(mine))

    def sb(name, shape, dt, dma=False):
        t = pes.enter_context(nc.sbuf_tensor(pfx + name, list(shape), dt))
        b = S.buf(pfx + name, t, dma=dma)
        mine.append(b)
        return b

    def ps(name, shape, dt=F32):
        t = pes.enter_context(nc.psum_tensor(pfx + name, list(shape), dt))
        return S.buf(pfx + name, t)

    return sb, ps


def load_consts(K, sb, names):
    nc, S, I = K.nc, K.S, K.I
    C = {}
    for nm in names:
        if nm == "ident_bf":
            C[nm] = sb("c_identb", [128, 128], BF16, dma=True)
            S.dma("pool", lambda e, b=C[nm]: e.dma_start(out=b[:], in_=I["c_ident"]), C[nm], w=[C[nm]])
        elif nm == "ident_f":
            C[nm] = sb("c_identf", [128, 128], F32, dma=True)
            S.dma("sp", lambda e, b=C[nm]: e.dma_start(out=b[:], in_=I["c_ident"]), C[nm], w=[C[nm]])
        elif nm == "trile_f":
            C[nm] = sb("c_trilef", [128, 128], F32, dma=True)
            S.dma("sp", lambda e, b=C[nm]: e.dma_start(out=b[:], in_=I["c_trile"]), C[nm], w=[C[nm]])
        elif nm == "ustr_bf":
            C[nm] = sb("c_ustrb", [128, 128], BF16, dma=True)
            S.dma("pool", lambda e, b=C[nm]: e.dma_start(out=b[:], in_=I["c_ustr"]), C[nm], w=[C[nm]])
        elif nm == "e3":
            C[nm] = sb("c_e3s", [16, 2048], F32, dma=True)
            S.dma("sp", lambda e, b=C[nm]: e.dma_start(out=b[:], in_=I["c_e3"]), C[nm], w=[C[nm]])
        elif nm == "iota":
            C[nm] = sb("c_iotas", [128, 1], F32, dma=True)
            S.dma("sp", lambda e, b=C[nm]: e.dma_start(out=b[:], in_=I["c_iota"]), C[nm], w=[C[nm]])
        elif nm == "bpos":
            C[nm] = sb("c_bposs", [128, K.NBLK], F32, dma=True)
            S.dma("sp", lambda e, b=C[nm]: e.dma_start(out=b[:], in_=I["c_bpos"]), C[nm], w=[C[nm]])
        elif nm == "neghalf":
            C[nm] = sb("c_neghalf", [128, 512], F32)
            S.op("pool", lambda e, b=C[nm]: e.memset(b[:], -0.5), w=[C[nm]])
        elif nm == "ones_bf":
            C[nm] = sb("c_onesb", [128, 128], BF16)
            S.op("pool", lambda e, b=C[nm]: e.memset(b[:], 1.0), w=[C[nm]])
        elif nm == "ones_f":
            C[nm] = sb("c_onesf", [128, 128], F32)
            S.op("pool", lambda e, b=C[nm]: e.memset(b[:], 1.0), w=[C[nm]])
        elif nm == "onesdiv_bf":
            C[nm] = sb("c_onesdiv", [128, 128], BF16)
            S.op("pool", lambda e, b=C[nm]: e.memset(b[:], 1.0 / 1024.0), w=[C[nm]])
    return C


def load_colvec(K, sb, name, src, nchunk, perm=False):
    nc, S = K.nc, K.S
    b = sb(name, [128, nchunk], F32, dma=True)
    if perm:
        S.dma("sp", lambda e: e.dma_start(out=b[:], in_=src.rearrange("(p c) -> p c", c=nchunk)), b, w=[b])
    else:
        with nc.allow_non_contiguous_dma(reason="small column vector"):
            S.dma("sp", lambda e: e.dma_start(out=b[:], in_=src.rearrange("(c p) -> p c", p=128)), b, w=[b])
    return b


def load_rowbc(K, sb, name, src, n):
    S = K.S
    b = sb(name, [128, n], F32, dma=True)
    S.dma("sp", lambda e: e.dma_start(out=b[:], in_=src.unsqueeze(0).partition_broadcast(128)), b, w=[b])
    return b


def load_weight_bf(K, sb, name, src, kin, nout, gvec=None, stage=None, perm=False, wb=None):
    nc, S = K.nc, K.S
    kc = kin // 128
    if wb is None:
        wb = sb(name, [128, kc, nout], BF16, dma=(gvec is None))
    if gvec is None:
        for c in range(kc):
            if perm:
                raise NotImplementedError
            S.dma("pool", lambda e, c=c: e.dma_start(out=wb[:, c, :], in_=src[c * 128:(c + 1) * 128, :]), wb, w=[wb])
        return wb
    i = 0
    for c in range(kc):
        for n0 in range(0, nout, 2048):
            n1 = min(nout, n0 + 2048)
            st = stage[i % len(stage)]
            if perm:
                srcap = src.rearrange("(p c) n -> p c n", c=kc)[:, c, n0:n1]
            else:
                srcap = src[c * 128:(c + 1) * 128, n0:n1]
            S.dma("sp", lambda e, st=st, srcap=srcap, n=n1 - n0: e.dma_start(out=st[:, 0:n], in_=srcap), st, w=[st])
            eng = "dve" if i % 2 == 0 else "act"
            if eng == "dve":
                S.op("dve", lambda e, st=st, c=c, n0=n0, n1=n1: e.tensor_scalar(
                    wb[:, c, n0:n1], st[:, 0:n1 - n0], gvec[:, c:c + 1], None, ALU.mult), r=[st, gvec], w=[wb])
            else:
                S.op("act", lambda e, st=st, c=c, n0=n0, n1=n1: e.activation(
                    out=wb[:, c, n0:n1], in_=st[:, 0:n1 - n0], func=AF.Copy, scale=gvec[:, c:c + 1]), r=[st, gvec], w=[wb])
            i += 1
    return wb


def rstd_from_ss(K, ss, rstd, v, neghalf, n=1024.0):
    S = K.S
    S.op("dve", lambda e: e.tensor_scalar(v[:, 0:1], ss[:, 0:1], 1.0 / n, EPS, ALU.mult, ALU.add), r=[ss], w=[v])
    S.op("pool", lambda e: e.tensor_tensor(rstd[:, 0:1], v[:, 0:1], neghalf[:, 0:1], ALU.pow), r=[v, neghalf], w=[rstd])


def phase_A(K, pes):
    nc, S, I, Dd = K.nc, K.S, K.I, K.Dd
    sb, ps = mk_alloc(K, pes)
    C = load_consts(K, sb, ["ident_bf", "neghalf"])
    gm = load_colvec(K, sb, "gm", I["norm_mix_g"], 8)
    winb = sb("winb", [128, 8, NIN], BF16)
    junk = sb("junkA", [128, D], BF16)
    with contextlib.ExitStack() as tmpes:
        sbt, _ = mk_alloc(K, tmpes)
        wst = [sbt(f"wst{i}", [128, 2048], F32, dma=True) for i in range(2)]
        load_weight_bf(K, sb, "winb", I["w_in"], D, NIN, gvec=gm, stage=wst, wb=winb)
        S.barrier()
    ident = C["ident_bf"]

    def streamA(sid, groups):
        def T(n, shape, dt, dma=False):
            return sb(f"{n}A{sid}", shape, dt, dma=dma)
        xt = [T(f"xt{i}", [128, D], F32, True) for i in range(2)]
        ss = [T(f"ss{i}", [128, 1], F32) for i in range(2)]
        vv = [T(f"vv{i}", [128, 1], F32) for i in range(2)]
        rstd = [T(f"rstd{i}", [128, 1], F32) for i in range(2)]
        ub = [T(f"ub{i}", [128, D], BF16) for i in range(8)]
        u = T("uT", [128, 8, 512], BF16)
        szt = [T(f"szt{i}", [128, D], BF16, True) for i in range(2)]
        dtt = [T(f"dtt{i}", [128, 16], F32, True) for i in range(2)]
        stg = [T(f"stg{i}", [128, 4, 512], BF16, True) for i in range(2)]
        sgb = [T(f"sgb{i}", [128, 512], BF16) for i in range(2)]
        pT = ps(f"pTA{sid}", [128, 8, 128], BF16)
        pz = ps(f"pzA{sid}", [128, 512])
        pf = [ps(f"pfA{sid}_{i}", [128, 512]) for i in range(2)]
        nf = 0
        nst = 0
        def emit_norm(gidx, g, j):
            t = g * 4 + j
            x_ = xt[t % 2]
            S.dma("sp", lambda e: e.dma_start(out=x_[:], in_=I["x"][t * 128:(t + 1) * 128, :]), x_, w=[x_])
            s_, v_, r_, ub_ = ss[t % 2], vv[t % 2], rstd[t % 2], ub[(gidx % 2) * 4 + j]
            S.op("act", lambda e: e.activation(out=junk[:], in_=x_[:], func=AF.Square, accum_out=s_[:, 0:1]),
                 r=[x_], w=[junk, s_])
            rstd_from_ss(K, s_, r_, v_, C["neghalf"])
            S.op("dve", lambda e: e.tensor_scalar(ub_[:], x_[:], r_[:, 0:1], None, ALU.mult), r=[x_, r_], w=[ub_])

        for j in range(4):
            emit_norm(0, groups[0], j)
        for gidx, g in enumerate(groups):
            for j in range(4):
                t = g * 4 + j
                ub_ = ub[(gidx % 2) * 4 + j]
                for c in range(8):
                    S.op("pe", lambda e, c=c, ub_=ub_: e.transpose(pT[:, c, :], ub_[:, c * 128:(c + 1) * 128], ident[:]),
                         r=[ub_, ident], w=[pT])
                S.op("act", lambda e, j=j: e.copy(u[:, :, j * 128:(j + 1) * 128], pT[:]), r=[pT], w=[u])
                yield
            for j in range(4):
                t = g * 4 + j
                sz_ = szt[t % 2]
                pTf = pT.t[:].bitcast(F32)
                for h in range(2):
                    pzb, pzap = (pz, pz[:]) if h == 0 else (pT, pTf)
                    for c in range(8):
                        S.op("pe", _mm(pzap, u[:, c, j * 128:(j + 1) * 128], winb[:, c, h * 512:(h + 1) * 512], c == 0, c == 7),
                             r=[u, winb], w=[pzb])
                    sgz = sgb[h]
                    S.op("act", lambda e, sgz=sgz, pzap=pzap: e.activation(out=sgz[:], in_=pzap, func=AF.Sigmoid), r=[pzb], w=[sgz])
                    S.op("dve", lambda e, h=h, sz_=sz_, sgz=sgz, pzap=pzap: e.tensor_tensor(sz_[:, h * 512:(h + 1) * 512], pzap, sgz[:], ALU.mult),
                         r=[pzb, sgz], w=[sz_])
                S.dma("sp", lambda e, sz_=sz_, t=t: e.dma_start(out=Dd["SZ"][t * 128:(t + 1) * 128, :], in_=sz_[:]), sz_, r=[sz_])
                for c in range(8):
                    S.op("pe", _mm(pz[:, 0:16], u[:, c, j * 128:(j + 1) * 128], winb[:, c, OFF_DT:OFF_DT + 16], c == 0, c == 7),
                         r=[u, winb], w=[pz])
                d_ = dtt[t % 2]
                S.op("dve", lambda e, d_=d_: e.tensor_copy(d_[:], pz[:, 0:16]), r=[pz], w=[d_])
                S.dma("sp", lambda e, d_=d_, t=t: e.dma_start(out=Dd["DTR"][t * 128:(t + 1) * 128, :], in_=d_[:]), d_, r=[d_])
                yield
            items = [("xbc", c) for c in range(12)] + [("glu", c) for c in range(8)] + [("gate", c) for c in range(16)]
            for idx, (kind, c) in enumerate(items):
                if gidx + 1 < len(groups) and idx in (4, 8, 12, 16):
                    emit_norm(gidx + 1, groups[gidx + 1], idx // 4 - 1)
                st = stg[nst % 2]
                slot = idx % 4

                def fm(col0):
                    nonlocal nf
                    pf_ = pf[nf % 2]
                    nf += 1
                    for kc in range(8):
                        S.op("pe", _mm(pf_[:], winb[:, kc, col0:col0 + 128], u[:, kc, :], kc == 0, kc == 7), r=[u, winb], w=[pf_])
                    return pf_

                if kind == "xbc":
                    pf_ = fm(OFF_XBC + c * 128)
                    if idx % 2 == 0:
                        S.op("act", lambda e, pf_=pf_, st=st, slot=slot: e.copy(st[:, slot, :], pf_[:]), r=[pf_], w=[st])
                    else:
                        S.op("dve", lambda e, pf_=pf_, st=st, slot=slot: e.tensor_copy(st[:, slot, :], pf_[:]), r=[pf_], w=[st])
                    dst, row0 = Dd["XBCT"], (c - slot) * 128
                elif kind == "glu":
                    pa = fm(OFF_GLU + c * 128)
                    pb = fm(OFF_GLU + D + c * 128)
                    sg_ = sgb[c % 2]
                    S.op("act", lambda e, pb=pb, sg_=sg_: e.activation(out=sg_[:], in_=pb[:], func=AF.Sigmoid), r=[pb], w=[sg_])
                    S.op("dve", lambda e, pa=pa, sg_=sg_, st=st, slot=slot: e.tensor_tensor(st[:, slot, :], pa[:], sg_[:], ALU.mult),
                         r=[pa, sg_], w=[st])
                    dst, row0 = Dd["GLUT"], (c - slot) * 128
                else:
                    pf_ = fm(OFF_GATE + c * 128)
                    S.op("act", lambda e, pf_=pf_, st=st, slot=slot: e.activation(out=st[:, slot, :], in_=pf_[:], func=AF.Sigmoid),
                         r=[pf_], w=[st])
                    dst, row0 = Dd["GATET"], (c - slot) * 128
                if slot == 3:
                    S.dma("sp", lambda e, st=st, dst=dst, row0=row0, g=g: e.dma_start(
                        out=dst[row0:row0 + 512, g * 512:(g + 1) * 512].rearrange("(c p) t -> p c t", p=128), in_=st[:]),
                        st, r=[st])
                    nst += 1
                yield

    gens = [streamA(0, list(range(0, K.NG, 2))), streamA(1, list(range(1, K.NG, 2)))]
    for _ in range(12):
        next(gens[0])
    run_streams(gens)


def make_consts(nblk):
    s = np.arange(128)
    c = {}
    c["c_ident"] = np.eye(128, dtype=np.float32)
    c["c_trile"] = (s[:, None] <= s[None, :]).astype(np.float32)
    c["c_ustr"] = (s[:, None] < s[None, :]).astype(np.float32)
    e3 = np.zeros((16, 16, 128), np.float32)
    for h in range(16):
        e3[h, h, :] = 1.0
    c["c_e3"] = e3.reshape(16, 2048)
    c["c_iota"] = s.astype(np.float32).reshape(128, 1)
    c["c_bpos"] = np.broadcast_to((np.arange(nblk) * float(MB)).astype(np.float32)[None, :], (128, nblk)).copy()
    return c


def make_in_map(inp, core, nseq):
    b0 = core * nseq
    T = nseq * SEQ
    nblk = (2 * T) // MB + NEXP
    m = {}
    m["x"] = np.ascontiguousarray(inp["x"][b0:b0 + nseq].reshape(T, D))
    m["p"] = np.ascontiguousarray(inp["p"][0, b0:b0 + nseq].reshape(T, 256))
    for k in ["norm_mix_g", "w_in", "ssd_conv_w", "ssd_conv_b", "ssd_dt_bias", "ssd_a_log", "ssd_d", "ssd_norm_g",
              "w_ssd_out", "conf_dw_w", "conf_dw_b", "conf_ln_g", "conf_ln_b", "w_conf_out", "w_o", "norm_ffn_g",
              "norm_ple_g", "w_ple_gate", "w_ple_proj"]:
        m[k] = np.ascontiguousarray(inp[k][0])
    m["final_norm_g"] = np.ascontiguousarray(inp["final_norm_g"])
    m["router_w"] = np.ascontiguousarray(np.concatenate([inp["router_group_w"][0], inp["router_expert_w"][0]], axis=1))
    m["router_b"] = np.ascontiguousarray(np.concatenate([inp["router_group_b"][0], inp["router_expert_b"][0]], axis=0))
    m["expert_w_gate"] = np.ascontiguousarray(inp["expert_w_gate"][0]).reshape(-1, 2048)
    m["expert_w_up"] = np.ascontiguousarray(inp["expert_w_up"][0]).reshape(-1, 2048)
    m["expert_w_down"] = np.ascontiguousarray(inp["expert_w_down"][0]).reshape(NEXP * DEXP, D)
    m.update(make_consts(nblk))
    return m


_NC_CACHE = {}


def kernel(**inputs):
    inp = {k: np.asarray(v) for k, v in inputs.items()}
    ncores = 8
    nseq = inp["x"].shape[0] // ncores
    if nseq not in _NC_CACHE:
        _NC_CACHE[nseq] = build(nseq)
    nc = _NC_CACHE[nseq]
    in_maps = [make_in_map(inp, c, nseq) for c in range(ncores)]
    res = run_bass_kernel_spmd(nc, in_maps, core_ids=list(range(ncores)))
    outs = [np.asarray(r["out"]).reshape(nseq, SEQ, D) for r in res.results]
    return np.concatenate(outs, axis=0).astype(np.float32)


def bc(ap, shape):
    return ap.to_broadcast(list(shape))


def run_streams(gens):
    gens = list(gens)
    while gens:
        for g_ in list(gens):
            try:
                next(g_)
            except StopIteration:
                gens.remove(g_)


def phase_B(K, pes):
    nc, S, I, Dd = K.nc, K.S, K.I, K.Dd
    sb, ps = mk_alloc(K, pes)
    C = load_consts(K, sb, ["ident_bf", "ident_f", "trile_f", "ones_f", "e3", "neghalf"])
    identb, identf, trile, onesf, e3 = C["ident_bf"], C["ident_f"], C["trile_f"], C["ones_f"], C["e3"]
    cw = sb("cwB", [128, 4, 12], F32, dma=True)
    with nc.allow_non_contiguous_dma(reason="small conv weights"):
        S.dma("sp", lambda e: e.dma_start(out=cw[:], in_=I["ssd_conv_w"].rearrange("k (c p) -> p k c", p=128)), cw, w=[cw])
    cbcol = load_colvec(K, sb, "cbcolB", I["ssd_conv_b"], 12)
    cbrow = sb("cbrowB", [1, 1536], BF16, dma=True)
    S.dma("pool", lambda e: e.dma_start(out=cbrow[:], in_=I["ssd_conv_b"].unsqueeze(0)), cbrow, w=[cbrow])
    ones1 = sb("ones1B", [1, 128], BF16)
    S.op("pool", lambda e: e.memset(ones1[:], 1.0), w=[ones1])
    diag = sb("diagB", [128, 48, 128], BF16)
    for k in range(4):
        S.op("dve", lambda e, k=k: e.tensor_tensor(diag[:, k * 12:(k + 1) * 12, :], bc(identf[:].unsqueeze(1), [128, 12, 128]),
             bc(cw[:, k, :].unsqueeze(2), [128, 12, 128]), ALU.mult), r=[identf, cw], w=[diag])
    dtb = load_rowbc(K, sb, "dtbB", I["ssd_dt_bias"], 16)
    alog = load_rowbc(K, sb, "alogB", I["ssd_a_log"], 16)
    drow = load_rowbc(K, sb, "drowB", I["ssd_d"], 16)
    arow = sb("arowB", [128, 16], F32)
    S.op("act", lambda e: e.activation(out=arow[:], in_=alog[:], func=AF.Exp), r=[alog], w=[arow])
    S.op("dve", lambda e: e.tensor_scalar(arow[:], arow[:], -1.0, None, ALU.mult), r=[arow], w=[arow])
    gssd = load_colvec(K, sb, "gssdB", I["ssd_norm_g"], 8)
    wssd = sb("wssd", [128, 8, D], BF16)
    junk = sb("junkB", [128, D], BF16)
    selb = sb("selbB", [48, 2048], BF16, dma=True)
    S.op("pool", lambda e: e.memset(selb[:], 0.0), w=[selb])
    S.dma("pool", lambda e: e.dma_start(out=selb[0:16, :], in_=I["c_e3"]), selb, w=[selb])
    S.dma("pool", lambda e: e.dma_start(out=selb[32:48, :], in_=I["c_e3"]), selb, w=[selb])
    with contextlib.ExitStack() as tmpes:
        sbt, _ = mk_alloc(K, tmpes)
        wst = [sbt(f"wstB{i}", [128, 2048], F32, dma=True) for i in range(2)]
        load_weight_bf(K, sb, "wssd", I["w_ssd_out"], D, D, gvec=gssd, stage=wst, wb=wssd)
        S.barrier()
    neghalf = C["neghalf"]

    def stream(sid, seqs):
        def T(n, shape, dt, dma=False):
            return sb(f"{n}B{sid}", shape, dt, dma=dma)
        xin = T("xin", [128, 12, 515], BF16, True)
        szt = T("szt", [128, D], BF16, True)
        gtn = [T(f"gtn{i}", [128, 512], BF16, True) for i in range(2)]
        dtr = T("dtr", [128, 4, 16], F32, True)
        BT = T("BT", [128, 2, 512], BF16)
        CT = T("CT", [128, 2, 512], BF16)
        ynT = T("ynT", [128, 8, 512], BF16)
        stg = T("stg", [128, 4, 512], BF16, True)
        xs_ = T("xs", [128, D], F32)
        bt_ = T("btok", [128, 256], BF16)
        sm = T("sm", [128, 8, 64], F32)
        acs = T("acs", [128, 128], F32)
        dApad = T("dApad", [128, 4, 48], F32)
        S.op("pool", lambda e: e.memset(dApad[:], 0.0), w=[dApad])
        aHL = T("aHL", [48, 512], BF16)
        S.op("pool", lambda e: e.memset(aHL[:], 0.0), w=[aHL])
        tH = T("tH", [48, 512], BF16)
        nHL = T("nHL", [48, 512], BF16)
        pvb = T("pvb", [128, D], F32)
        segq = [T(f"segq{i}", [128, 512], F32) for i in range(2)]
        dm = T("dm", [128, 16, 128], BF16)
        gm_ = T("gm", [128, 2, 128], BF16)
        Mt = T("Mt", [128, 16, 128], BF16)
        xdt = T("xdt", [128, 16, 64], BF16)
        xdtd = T("xdtd", [128, 16, 64], BF16)
        y1 = T("y1", [128, D], F32)
        ysk = T("ysk", [128, D], BF16)
        ss = T("ss", [128, 1], F32)
        vv = T("vv", [128, 1], F32)
        rstd = T("rstd", [128, 1], F32)
        yn = T("yn", [128, D], BF16)
        prev = T("prev", [128, D], F32)
        prevb = T("prevb", [128, D], BF16)
        q = [ps(f"qB{sid}_{i}", [128, 512]) for i in range(4)]
        q2b = q[2].t[:].bitcast(BF16).rearrange("p (c t) -> p c t", c=8)
        smv = lambda r: sm[:, r, :].rearrange("p (j h) -> p j h", j=4)

        for sq in seqs:
            for gi in range(4):
                g = sq * 4 + gi
                t0 = g * 512
                if gi == 0:
                    S.op("pool", lambda e: e.memset(xin[:, :, 0:3], 0.0), w=[xin])
                    S.dma("sp", lambda e: e.dma_start(out=xin[:, :, 3:515],
                          in_=Dd["XBCT"][:, t0:t0 + 512].rearrange("(c p) t -> p c t", p=128)), xin, w=[xin])
                    S.op("pool", lambda e: e.memset(prev[:], 0.0), w=[prev])
                    S.op("pool", lambda e: e.memset(prevb[:], 0.0), w=[prevb])
                else:
                    S.dma("sp", lambda e: e.dma_start(out=xin[:],
                          in_=Dd["XBCT"][:, t0 - 3:t0 + 512].rearrange("(c p) t -> p c t", p=128)), xin, w=[xin])
                S.dma("sp", lambda e: e.dma_start(out=dtr[:], in_=Dd["DTR"][t0:t0 + 512, :].rearrange("(j p) h -> p j h", p=128)),
                      dtr, w=[dtr])
                for c in range(8, 12):
                    pl = q[c - 8]
                    for k in range(4):
                        S.op("pe", _mm(pl[:], diag[:, k * 12 + c, :], xin[:, c, k:k + 512], k == 0, k == 3), r=[diag, xin], w=[pl])
                    dst = BT if c < 10 else CT
                    S.op("act", lambda e, pl=pl, dst=dst, c=c: e.activation(out=dst[:, c % 2, :], in_=pl[:], func=AF.Silu,
                         bias=cbcol[:, c:c + 1]), r=[pl, cbcol], w=[dst])
                yield
                S.op("dve", lambda e: e.tensor_tensor(smv(0), dtr[:], bc(dtb[:].unsqueeze(1), [128, 4, 16]), ALU.add), r=[dtr, dtb], w=[sm])
                S.op("act", lambda e: e.activation(out=sm[:, 1, :], in_=sm[:, 0, :], func=AF.Exp), r=[sm], w=[sm])
                S.op("act", lambda e: e.activation(out=sm[:, 2, :], in_=sm[:, 1, :], func=AF.Ln, bias=1.0), r=[sm], w=[sm])
                S.op("dve", lambda e: e.tensor_tensor(smv(3), smv(2), bc(arow[:].unsqueeze(1), [128, 4, 16]), ALU.mult), r=[sm, arow], w=[sm])
                S.op("pe", _mm(q[0][:, 0:64], trile[:], sm[:, 3, :], True, True), r=[trile, sm], w=[q[0]])
                S.op("pe", _mm(q[0][:, 64:128], onesf[:], sm[:, 3, :], True, True), r=[onesf, sm], w=[q[0]])
                S.op("dve", lambda e: e.tensor_copy(dApad[:, :, 0:16], smv(3)), r=[sm], w=[dApad])
                S.op("dve", lambda e: e.tensor_copy(dApad[:, :, 32:48], smv(3)), r=[sm], w=[dApad])
                for j in range(4):
                    S.op("pe", _mm(q[1][0:48, j * 128:(j + 1) * 128], dApad[:, j, :], trile[:], True, True),
                         r=[trile, dApad], w=[q[1]])
                S.op("dve", lambda e: e.tensor_copy(acs[:], q[0][:, 0:128]), r=[q[0]], w=[acs])
                S.op("dve", lambda e: e.tensor_copy(aHL[0:16, :], q[1][0:16, :]), r=[q[1]], w=[aHL])
                S.op("dve", lambda e: e.tensor_copy(tH[32:48, :], q[1][32:48, :]), r=[q[1]], w=[tH])
                S.op("dve", lambda e: e.tensor_tensor(aHL[32:48, :], q[1][32:48, :], tH[32:48, :], ALU.subtract), r=[q[1], tH], w=[aHL])
                S.op("dve", lambda e: e.tensor_scalar(nHL[:], aHL[:], -1.0, None, ALU.mult), r=[aHL], w=[nHL])
                S.op("act", lambda e: e.activation(out=sm[:, 4, :], in_=acs[:, 0:64], func=AF.Exp), r=[acs], w=[sm])
                S.op("dve", lambda e: e.tensor_tensor(sm[:, 0, :], acs[:, 64:128], acs[:, 0:64], ALU.subtract), r=[acs], w=[sm])
                S.op("act", lambda e: e.activation(out=sm[:, 5, :], in_=sm[:, 0, :], func=AF.Exp), r=[sm], w=[sm])
                S.op("act", lambda e: e.activation(out=sm[:, 6, :], in_=acs[:, 64:128], func=AF.Exp), r=[acs], w=[sm])
                S.op("dve", lambda e: e.tensor_tensor(sm[:, 7, :], sm[:, 2, :], sm[:, 5, :], ALU.mult), r=[sm], w=[sm])
                yield
                for j in range(4):
                    cj = j * 128
                    tt = g * 4 + j
                    hs = slice(j * 16, (j + 1) * 16)
                    S.dma("sp", lambda e: e.dma_start(out=szt[:], in_=Dd["SZ"][tt * 128:(tt + 1) * 128, :]), szt, w=[szt])
                    for hf in range(2):
                        for c4 in range(4):
                            c = 4 * hf + c4
                            o = q[hf][:, c4 * 128:(c4 + 1) * 128]
                            for k in range(4):
                                S.op("pe", _mm(o, xin[:, c, cj + k:cj + k + 128], diag[:, k * 12 + c, :], k == 0, False),
                                     r=[xin, diag], w=[q[hf]])
                            S.op("pe", _mm(o, ones1[0:1, :], cbrow[0:1, c * 128:(c + 1) * 128], False, True), r=[ones1, cbrow], w=[q[hf]])
                        S.op("act", lambda e, hf=hf: e.activation(out=xs_[:, hf * 512:(hf + 1) * 512], in_=q[hf][:], func=AF.Silu),
                             r=[q[hf]], w=[xs_])
                    for c in (8, 9):
                        o = q[2][:, (c - 8) * 128:(c - 7) * 128]
                        for k in range(4):
                            S.op("pe", _mm(o, xin[:, c, cj + k:cj + k + 128], diag[:, k * 12 + c, :], k == 0, False), r=[xin, diag], w=[q[2]])
                        S.op("pe", _mm(o, ones1[0:1, :], cbrow[0:1, c * 128:(c + 1) * 128], False, True), r=[ones1, cbrow], w=[q[2]])
                    S.op("act", lambda e: e.activation(out=bt_[:], in_=q[2][:, 0:256], func=AF.Silu), r=[q[2]], w=[bt_])
                    for g2 in range(2):
                        S.op("pe", _mm(q[2][:, 256 + g2 * 128:384 + g2 * 128], BT[:, g2, cj:cj + 128], CT[:, g2, cj:cj + 128], True, True),
                             r=[BT, CT], w=[q[2]])
                    S.op("dve", lambda e: e.tensor_tensor(gm_[:], q[2][:, 256:512].rearrange("p (g l) -> p g l", g=2),
                         bc(trile[:].unsqueeze(1), [128, 2, 128]), ALU.mult), r=[q[2], trile], w=[gm_])
                    yield
                    xs3 = xs_[:].rearrange("p (h d) -> p h d", h=16)
                    S.op("dve", lambda e: e.tensor_tensor(xdt[:], xs3, bc(sm[:, 2, hs].unsqueeze(2), [128, 16, 64]), ALU.mult),
                         r=[xs_, sm], w=[xdt])
                    S.op("pool", lambda e: e.tensor_tensor(xdtd[:], xs3, bc(sm[:, 7, hs].unsqueeze(2), [128, 16, 64]), ALU.mult),
                         r=[xs_, sm], w=[xdtd])
                    S.op("pool", lambda e: e.tensor_tensor(ysk[:].rearrange("p (h d) -> p h d", h=16), xs3,
                         bc(drow[:].unsqueeze(2), [128, 16, 64]), ALU.mult), r=[xs_, drow], w=[ysk])
                    yield
                    for qq in range(4):
                        sg_ = segq[qq % 2]
                        pl = q[3] if qq % 2 == 0 else q[0]
                        for h4 in range(4):
                            hsel = (4 * qq + h4) * 128
                            S.op("pe", _mm(pl[:, h4 * 128:(h4 + 1) * 128], selb[:, hsel:hsel + 128], aHL[:, cj:cj + 128], h4 == 0, False, True),
                                 r=[selb, aHL], w=[pl])
                        S.op("pe", _mm(pl[:], nHL[:, cj:cj + 128], selb[:, qq * 512:(qq + 1) * 512], False, True, True), r=[selb, nHL], w=[pl])
                        S.op("dve", lambda e, sg_=sg_, pl=pl: e.tensor_scalar(sg_[:], pl[:], 0.0, None, ALU.min), r=[pl], w=[sg_])
                        S.op("act", lambda e, sg_=sg_, qq=qq: e.activation(out=dm[:, 4 * qq:4 * qq + 4, :].rearrange("p h l -> p (h l)"),
                             in_=sg_[:], func=AF.Exp), r=[sg_], w=[dm])
                        yield
                    for g2 in range(2):
                        S.op("dve", lambda e, g2=g2: e.tensor_tensor(Mt[:, 8 * g2:8 * g2 + 8, :], dm[:, 8 * g2:8 * g2 + 8, :],
                             bc(gm_[:, g2:g2 + 1, :], [128, 8, 128]), ALU.mult), r=[dm, gm_], w=[Mt])
                    for hf in range(2):
                        for h8 in range(8):
                            h = 8 * hf + h8
                            S.op("pe", _mm(q[2 + hf][:, h8 * 64:(h8 + 1) * 64], Mt[:, h, :], xdt[:, h, :], h8 == 0, False, True),
                                 r=[Mt, xdt], w=[q[2 + hf]])
                        S.op("pe", _mm(q[2 + hf][:], identb[:], ysk[:, hf * 512:(hf + 1) * 512], False, True, True), r=[identb, ysk], w=[q[2 + hf]])
                    for hf in range(2):
                        S.op("pe", _mm(q[hf][:], CT[:, hf, cj:cj + 128], prevb[:, hf * 512:(hf + 1) * 512], True, True),
                             r=[CT, prevb], w=[q[hf]])
                    yield
                    for hf in range(2):
                        sl = slice(hf * 512, (hf + 1) * 512)
                        S.op("dve", lambda e, hf=hf, sl=sl: e.tensor_tensor(y1[:, sl].rearrange("p (h d) -> p h d", h=8),
                             q[hf][:].rearrange("p (h d) -> p h d", h=8),
                             bc(sm[:, 4, j * 16 + 8 * hf:j * 16 + 8 * hf + 8].unsqueeze(2), [128, 8, 64]), ALU.mult),
                             r=[q[hf], sm], w=[y1])
                        S.op("dve", lambda e, hf=hf, sl=sl: e.tensor_tensor(y1[:, sl], q[2 + hf][:], y1[:, sl], ALU.add),
                             r=[q[2 + hf], y1], w=[y1])
                    S.op("dve", lambda e: e.tensor_tensor(y1[:], y1[:], szt[:], ALU.mult), r=[y1, szt], w=[y1])
                    yield
                    for hf in range(2):
                        S.op("pe", _mm(q[hf][:], bt_[:, hf * 128:(hf + 1) * 128],
                             xdtd[:, 8 * hf:8 * hf + 8, :].rearrange("p h d -> p (h d)"), True, True), r=[bt_, xdtd], w=[q[hf]])
                    S.op("pool", lambda e: e.tensor_tensor(pvb[:].rearrange("p (h d) -> p h d", h=16),
                         prev[:].rearrange("p (h d) -> p h d", h=16), bc(sm[:, 6, hs].unsqueeze(2), [128, 16, 64]), ALU.mult),
                         r=[prev, sm], w=[pvb])
                    for hf in range(2):
                        sl = slice(hf * 512, (hf + 1) * 512)
                        S.op("dve", lambda e, hf=hf, sl=sl: e.tensor_tensor(prev[:, sl], pvb[:, sl], q[hf][:], ALU.add),
                             r=[pvb, q[hf]], w=[prev])
                    S.op("act", lambda e: e.copy(prevb[:], prev[:]), r=[prev], w=[prevb])
                    yield
                    S.op("act", lambda e: e.activation(out=junk[:], in_=y1[:], func=AF.Square, accum_out=ss[:, 0:1]), r=[y1], w=[junk, ss])
                    rstd_from_ss(K, ss, rstd, vv, neghalf)
                    S.op("act", lambda e: e.activation(out=yn[:], in_=y1[:], func=AF.Copy, scale=rstd[:, 0:1]), r=[y1, rstd], w=[yn])
                    for c in range(8):
                        S.op("pe", lambda e, c=c: e.transpose(q2b[:, c, :], yn[:, c * 128:(c + 1) * 128], identb[:]), r=[yn, identb], w=[q[2]])
                    S.op("act", lambda e: e.copy(ynT[:, :, cj:cj + 128], q2b), r=[q[2]], w=[ynT])
                    yield
                for n in range(8):
                    po = q[n % 4]
                    gt_ = gtn[n % 2]
                    S.dma("sp", lambda e, gt_=gt_, n=n: e.dma_start(out=gt_[:], in_=Dd["GATET"][n * 128:(n + 1) * 128, t0:t0 + 512]),
                          gt_, w=[gt_])
                    for kc in range(8):
                        S.op("pe", _mm(po[:], wssd[:, kc, n * 128:(n + 1) * 128], ynT[:, kc, :], kc == 0, kc == 7), r=[wssd, ynT], w=[po])
                    S.op("dve", lambda e, po=po, n=n, gt_=gt_: e.tensor_tensor(stg[:, n % 4, :], po[:], gt_[:], ALU.mult),
                         r=[po, gt_], w=[stg])
                    if n % 4 == 3:
                        r0 = (n - 3) * 128
                        S.dma("sp", lambda e, r0=r0: e.dma_start(
                            out=Dd["GS1T"][r0:r0 + 512, t0:t0 + 512].rearrange("(c p) t -> p c t", p=128), in_=stg[:]), stg, r=[stg])
                    yield

    ns = K.nseq
    if ns >= 2:
        half = ns // 2
        gens = [stream(0, list(range(0, half))), stream(1, list(range(half, ns)))]
        for _ in range(B_OFFSET):
            next(gens[0])
    else:
        gens = [stream(0, [0])]
    run_streams(gens)


def phase_CDE(K, pes, upto):
    nc, S, I, Dd = K.nc, K.S, K.I, K.Dd
    NT, NBLK = K.NT, K.NBLK
    sbP, _ = mk_alloc(K, pes)
    CP = load_consts(K, sbP, ["neghalf", "iota", "bpos", "ident_bf"])
    neghalf, identb = CP["neghalf"], CP["ident_bf"]
    lgall = sbP("lgall", [128, NT, 36], F32)
    d1i = sbP("d1i", [128, 2, NT], I32)
    idxw = sbP("idxw", [128, 6, NBLK], I32)
    gw = sbP("gw", [128, 2, NT], F32)
    with contextlib.ExitStack() as pc:
        phase_C(K, pc, dict(lgall=lgall, neghalf=neghalf, identb=identb))
        S.barrier()
    if upto < "D":
        return
    with contextlib.ExitStack() as pr_:
        sbR, _ = mk_alloc(K, pr_)
        oh1 = sbR("oh1", [128, NT, 32], BF16)
        oh2 = sbR("oh2", [128, NT, 32], BF16)
        rk = sbR("rk", [128, 2, NT], F32)
        run = sbR("run", [128, 32], F32)
        with contextlib.ExitStack() as pc2:
            phase_C2(K, pc2, dict(lgall=lgall, oh1=oh1, oh2=oh2, rk=rk, gw=gw, run=run))
            S.barrier()
        with contextlib.ExitStack() as pd:
            phase_D(K, pd, dict(oh1=oh1, oh2=oh2, rk=rk, gw=gw, run=run, d1i=d1i, idxw=idxw, iota=CP["iota"], bpos=CP["bpos"],
                                identb=identb))
            S.barrier()
    if upto < "E":
        return
    with contextlib.ExitStack() as pe_:
        phase_E(K, pe_, dict(gw=gw, d1i=d1i, neghalf=neghalf, identb=identb))


def phase_C(K, pes, P):
    nc, S, I, Dd = K.nc, K.S, K.I, K.Dd
    identb, neghalf, lgall = P["identb"], P["neghalf"], P["lgall"]
    with contextlib.ExitStack() as es1:
        sb, ps = mk_alloc(K, es1)
        C = load_consts(K, sb, ["ident_f", "onesdiv_bf"])
        identf, onesdiv = C["ident_f"], C["onesdiv_bf"]
        cw = sb("cw31", [128, 31, 8], F32, dma=True)
        with nc.allow_non_contiguous_dma(reason="small conv weights"):
            S.dma("sp", lambda e: e.dma_start(out=cw[:], in_=I["conf_dw_w"].rearrange("k (c p) -> p k c", p=128)), cw, w=[cw])
        diag = sb("diag31", [128, 248, 128], BF16)
        for k in range(31):
            S.op("dve", lambda e, k=k: e.tensor_tensor(diag[:, k * 8:(k + 1) * 8, :],
                 bc(identf[:].unsqueeze(1), [128, 8, 128]), bc(cw[:, k, :].unsqueeze(2), [128, 8, 128]), ALU.mult), r=[identf, cw], w=[diag])
        cb31 = load_colvec(K, sb, "cb31", I["conf_dw_b"], 8)
        lng = load_colvec(K, sb, "lng", I["conf_ln_g"], 8)
        lnb = load_colvec(K, sb, "lnb", I["conf_ln_b"], 8)
        wconf = load_weight_bf(K, sb, "wconf", I["w_conf_out"], D, D)

        def stream1(sid, groups):
            def T(n, shape, dt, dma=False):
                return sb(f"{n}C{sid}", shape, dt, dma=dma)
            gl = T("glu", [128, 8, 542], BF16, True)
            gcn = [T(f"gcn{i}", [128, 512], BF16, True) for i in range(2)]
            g1n = [T(f"g1n{i}", [128, 512], BF16, True) for i in range(2)]
            cT = T("cT", [128, 8, 512], BF16)
            csq = [T(f"csq{i}", [128, 512], BF16) for i in range(2)]
            mean = T("mean", [128, 512], F32)
            rstdb = T("rstdb", [128, 512], F32)
            cn = [T(f"cn{i}", [128, 512], F32) for i in range(2)]
            var = cn[0]
            csT = T("csT", [128, 8, 512], BF16)
            stg = T("stg", [128, 4, 512], BF16, True)
            pc_ = ps(f"pcC{sid}", [128, 512])
            pm = [ps(f"pmC{sid}_{i}", [128, 512]) for i in range(2)]
            po = ps(f"poC{sid}", [128, 512])
            for g in groups:
                gi = g % 4
                t0 = g * 512
                if gi == 0:
                    S.op("pool", lambda e: e.memset(gl[:, :, 0:30], 0.0), w=[gl])
                    S.dma("sp", lambda e: e.dma_start(out=gl[:, :, 30:542],
                          in_=Dd["GLUT"][:, t0:t0 + 512].rearrange("(c p) t -> p c t", p=128)), gl, w=[gl])
                else:
                    S.dma("sp", lambda e: e.dma_start(out=gl[:],
                          in_=Dd["GLUT"][:, t0 - 30:t0 + 512].rearrange("(c p) t -> p c t", p=128)), gl, w=[gl])
                for c in range(8):
                    for k in range(31):
                        S.op("pe", _mm(pc_[:], diag[:, k * 8 + c, :], gl[:, c, k:k + 512], k == 0, k == 30), r=[diag, gl], w=[pc_])
                    S.op("act", lambda e, c=c: e.activation(out=cT[:, c, :], in_=pc_[:], func=AF.Identity, bias=cb31[:, c:c + 1]),
                         r=[pc_, cb31], w=[cT])
                    cq = csq[c % 2]
                    S.op("dve", lambda e, c=c, cq=cq: e.tensor_tensor(cq[:], cT[:, c, :], cT[:, c, :], ALU.mult), r=[cT], w=[cq])
                    S.op("pe", _mm(pm[0][:], onesdiv[:], cT[:, c, :], c == 0, c == 7), r=[onesdiv, cT], w=[pm[0]])
                    S.op("pe", _mm(pm[1][:], onesdiv[:], cq[:], c == 0, c == 7), r=[onesdiv, cq], w=[pm[1]])
                    yield
                S.op("dve", lambda e: e.tensor_copy(mean[:], pm[0][:]), r=[pm[0]], w=[mean])
                S.op("dve", lambda e: e.tensor_tensor(var[:], mean[:], mean[:], ALU.mult), r=[mean], w=[var])
                S.op("dve", lambda e: e.tensor_tensor(var[:], pm[1][:], var[:], ALU.subtract), r=[pm[1], var], w=[var])
                S.op("dve", lambda e: e.tensor_scalar(var[:], var[:], 0.0, EPS, ALU.max, ALU.add), r=[var], w=[var])
                S.op("act", lambda e: e.activation(out=rstdb[:], in_=var[:], func=AF.Ln), r=[var], w=[rstdb])
                S.op("act", lambda e: e.activation(out=rstdb[:], in_=rstdb[:], func=AF.Exp, scale=-0.5), r=[rstdb], w=[rstdb])
                yield
                for c in range(8):
                    cn_ = cn[c % 2]
                    S.op("dve", lambda e, c=c, cn_=cn_: e.tensor_tensor(cn_[:], cT[:, c, :], mean[:], ALU.subtract), r=[cT, mean], w=[cn_])
                    S.op("dve", lambda e, cn_=cn_: e.tensor_tensor(cn_[:], cn_[:], rstdb[:], ALU.mult), r=[cn_, rstdb], w=[cn_])
                    S.op("act", lambda e, c=c, cn_=cn_: e.activation(out=csT[:, c, :], in_=cn_[:], func=AF.Silu, bias=lnb[:, c:c + 1],
                         scale=lng[:, c:c + 1]), r=[cn_, lnb, lng], w=[csT])
                    if c % 2 == 1:
                        yield
                for n in range(8):
                    gc_, g1_ = gcn[n % 2], g1n[n % 2]
                    S.dma("sp", lambda e, gc_=gc_, n=n: e.dma_start(out=gc_[:], in_=Dd["GATET"][D + n * 128:D + (n + 1) * 128, t0:t0 + 512]),
                          gc_, w=[gc_])
                    S.dma("sp", lambda e, g1_=g1_, n=n: e.dma_start(out=g1_[:], in_=Dd["GS1T"][n * 128:(n + 1) * 128, t0:t0 + 512]),
                          g1_, w=[g1_])
                    for kc in range(8):
                        S.op("pe", _mm(po[:], wconf[:, kc, n * 128:(n + 1) * 128], csT[:, kc, :], kc == 0, kc == 7), r=[wconf, csT], w=[po])
                    tf = cn[n % 2]
                    S.op("dve", lambda e, tf=tf, gc_=gc_: e.tensor_tensor(tf[:], po[:], gc_[:], ALU.mult), r=[po, gc_], w=[tf])
                    S.op("dve", lambda e, tf=tf, n=n, g1_=g1_: e.tensor_tensor(stg[:, n % 4, :], tf[:], g1_[:], ALU.add),
                         r=[tf, g1_], w=[stg])
                    if n % 4 == 3:
                        r0 = (n - 3) * 128
                        S.dma("sp", lambda e, r0=r0: e.dma_start(
                            out=Dd["GST"][r0:r0 + 512, t0:t0 + 512].rearrange("(c p) t -> p c t", p=128), in_=stg[:]), stg, r=[stg])
                    yield

        gens = [stream1(0, list(range(0, K.NG, 2))), stream1(1, list(range(1, K.NG, 2)))]
        for _ in range(C1_OFFSET):
            next(gens[0])
        run_streams(gens)
        S.barrier()

    with contextlib.ExitStack() as es2:
        sb, ps = mk_alloc(K, es2)
        wo = load_weight_bf(K, sb, "wo", I["w_o"], D, D)
        gffn = load_rowbc(K, sb, "gffn", I["norm_ffn_g"], D)
        wrh = sb("wrh", [128, 8, 36], BF16)
        wrl = sb("wrl", [128, 8, 36], BF16)
        wr0 = sb("wr0", [128, 8, 36], F32, dma=True)
        S.dma("sp", lambda e: e.dma_start(out=wr0[:], in_=I["router_w"].rearrange("(p j) e -> p j e", j=8)), wr0, w=[wr0])
        S.op("dve", lambda e: e.tensor_copy(wrh[:], wr0[:]), r=[wr0], w=[wrh])
        S.op("dve", lambda e: e.tensor_tensor(wrl[:], wr0[:], wrh[:], ALU.subtract), r=[wr0, wrh], w=[wrl])
        rb = load_rowbc(K, sb, "rb", I["router_b"], 36)
        junk = sb("junkC", [128, D], BF16)

        def stream2(sid, tiles):
            def T(n, shape, dt, dma=False):
                return sb(f"{n}D{sid}", shape, dt, dma=dma)
            xL = [T(f"xt{i}", [128, D], F32, True) for i in range(2)]
            gL = [T(f"gst{i}", [128, 8, 128], BF16, True) for i in range(2)]
            h_ = T("h1t", [128, D], F32, True)
            u2_ = T("u2f", [128, D], F32)
            hi_ = T("hi", [128, D], BF16, True)
            lo_ = T("lo", [128, D], BF16)
            hT_ = T("hiT", [128, 8, 128], BF16)
            lT_ = T("loT", [128, 8, 128], BF16)
            ss_ = T("ss", [128, 1], F32)
            vv_ = T("vv", [128, 1], F32)
            rs_ = T("rs", [128, 1], F32)
            pA_ = ps(f"poD{sid}", [128, 512])
            pr_ = ps(f"prD{sid}", [128, 512])
            po = [pA_, pA_]
            pl = pA_
            prb_ = pr_.t[:].bitcast(BF16).rearrange("p (c t) -> p c t", c=8)
            def loads(i):
                t = tiles[i]
                x_, gst = xL[i % 2], gL[i % 2]
                S.dma("sp", lambda e: e.dma_start(out=x_[:], in_=I["x"][t * 128:(t + 1) * 128, :]), x_, w=[x_])
                S.dma("sp", lambda e: e.dma_start(out=gst[:], in_=Dd["GST"][:, t * 128:(t + 1) * 128].rearrange("(c p) t -> p c t", p=128)),
                      gst, w=[gst])

            loads(0)
            for i, t in enumerate(tiles):
                x_, gst = xL[i % 2], gL[i % 2]
                if i + 1 < len(tiles):
                    loads(i + 1)
                for h in range(2):
                    for kc in range(8):
                        S.op("pe", _mm(po[h][:], gst[:, kc, :], wo[:, kc, h * 512:(h + 1) * 512], kc == 0, kc == 7), r=[gst, wo], w=[po[h]])
                    S.op("dve", lambda e, h=h: e.tensor_tensor(h_[:, h * 512:(h + 1) * 512], po[h][:], x_[:, h * 512:(h + 1) * 512], ALU.add),
                         r=[po[h], x_], w=[h_])
                S.dma("sp", lambda e: e.dma_start(out=Dd["H1"][t * 128:(t + 1) * 128, :], in_=h_[:]), h_, r=[h_])
                yield
                S.op("act", lambda e: e.activation(out=junk[:], in_=h_[:], func=AF.Square, accum_out=ss_[:, 0:1]), r=[h_], w=[junk, ss_])
                rstd_from_ss(K, ss_, rs_, vv_, neghalf)
                S.op("dve", lambda e: e.scalar_tensor_tensor(u2_[:], h_[:], rs_[:, 0:1], gffn[:], ALU.mult, ALU.mult),
                     r=[h_, rs_, gffn], w=[u2_])
                yield
                u2p = u2_[:].rearrange("t (p j) -> t j p", j=8)
                S.op("act", lambda e: e.copy(hi_[:].rearrange("t (j p) -> t j p", j=8), u2p), r=[u2_], w=[hi_])
                S.op("dve", lambda e: e.tensor_tensor(lo_[:].rearrange("t (j p) -> t j p", j=8), u2p,
                     hi_[:].rearrange("t (j p) -> t j p", j=8), ALU.subtract), r=[u2_, hi_], w=[lo_])
                S.dma("sp", lambda e: e.dma_start(out=Dd["U2"][t * 128:(t + 1) * 128, :], in_=hi_[:]), hi_, r=[hi_])
                yield
                for src, dstT in ((hi_, hT_), (lo_, lT_)):
                    for c in range(8):
                        S.op("pe", lambda e, c=c, src=src: e.transpose(prb_[:, c, :], src[:, c * 128:(c + 1) * 128], identb[:]),
                             r=[src, identb], w=[pr_])
                    S.op("act", lambda e, dstT=dstT: e.copy(dstT[:], prb_), r=[pr_], w=[dstT])
                    yield
                n_mm = 0
                for (aT, wb_) in ((hT_, wrh), (lT_, wrh), (hT_, wrl)):
                    for c in range(8):
                        S.op("pe", _mm(pl[:, 0:36], aT[:, c, :], wb_[:, c, :], n_mm == 0, n_mm == 23), r=[aT, wb_], w=[pl])
                        n_mm += 1
                S.op("dve", lambda e: e.tensor_tensor(lgall[:, t, :], pl[:, 0:36], rb[:], ALU.add), r=[pl, rb], w=[lgall])
                yield

        gens = [stream2(i, list(range(i, K.NT, 4))) for i in range(4)]
        for i in range(3):
            for _ in range(2 * (3 - i)):
                next(gens[i])
        run_streams(gens)
        S.barrier()


def phase_C2(K, pes, P):
    nc, S, I, Dd = K.nc, K.S, K.I, K.Dd
    NT = K.NT
    sb, ps = mk_alloc(K, pes)
    C = load_consts(K, sb, ["ones_bf", "ustr_bf"])
    onesb, ustr = C["ones_bf"], C["ustr_bf"]
    lg, oh1, oh2, rk, gw, run = (P[k] for k in ("lgall", "oh1", "oh2", "rk", "gw", "run"))
    sc = sb("scR", [128, 10, NT], F32)
    gmask = sb("gmaskR", [128, NT, 4], F32)
    g4 = sb("g4R", [128, NT, 4], F32)
    ein = sb("einR", [128, NT, 8], F32)
    ein2 = sb("ein2R", [128, NT, 8], F32)
    t8 = sb("t8R", [128, NT, 8], F32)
    m1k = sb("m1kR", [128, NT, 8], F32)
    m2k = sb("m2kR", [128, NT, 8], F32)
    ohs = sb("ohsR", [128, NT, 32], BF16)
    Pf = sb("PfR", [128, NT, 32], F32)
    cntf = sb("cntfR", [128, NT, 32], F32)
    base = sb("baseR", [128, NT, 32], F32)
    tmp = sb("tmpR", [128, NT, 32], F32)
    nq = (NT * 32 + 511) // 512
    pp = [ps(f"ppR{i}", [128, 512]) for i in range(min(nq, 4))]
    pcn = [ps(f"pcR{i}", [128, 512]) for i in range(min(nq, 4))]
    lgG = lg[:, :, 0:4]
    key8 = sb("key8R", [128, NT, 8], F32)
    for e_ in range(8):
        S.op("pool", lambda e, e_=e_: e.memset(key8[:, :, e_:e_ + 1], float(8 - e_)), w=[key8])
    kt = sb("ktR", [128, NT, 8], F32)
    kmx = sb("kmxR", [128, NT], F32)

    def first_only(mask, n):
        S.op("dve", lambda e: e.tensor_tensor(kt[:, :, 0:n], mask[:], key8[:, :, 0:n], ALU.mult), r=[mask, key8], w=[kt])
        S.op("dve", lambda e: e.reduce_max(kmx[:], kt[:, :, 0:n], AX.X), r=[kt], w=[kmx])
        S.op("dve", lambda e: e.tensor_tensor(mask[:], kt[:, :, 0:n], bc(kmx[:].unsqueeze(2), [128, NT, n]), ALU.is_equal),
             r=[kt, kmx], w=[mask])

    S.op("dve", lambda e: e.reduce_max(sc[:, 0, :], lgG, AX.X), r=[lg], w=[sc])
    S.op("dve", lambda e: e.tensor_tensor(gmask[:], lgG, bc(sc[:, 0, :].unsqueeze(2), [128, NT, 4]), ALU.is_equal), r=[lg, sc], w=[gmask])
    first_only(gmask, 4)
    S.op("dve", lambda e: e.tensor_tensor(g4[:], lgG, bc(sc[:, 0, :].unsqueeze(2), [128, NT, 4]), ALU.subtract), r=[lg, sc], w=[g4])
    S.op("act", lambda e: e.activation(out=g4[:], in_=g4[:], func=AF.Exp), r=[g4], w=[g4])
    S.op("dve", lambda e: e.reduce_sum(sc[:, 1, :], g4[:], AX.X), r=[g4], w=[sc])
    S.op("dve", lambda e: e.reciprocal(sc[:, 2, :], sc[:, 1, :]), r=[sc], w=[sc])
    lgE = lg[:, :, 4:36].rearrange("p t (g e) -> p t g e", g=4)
    for g in range(4):
        dst = ein if g == 0 else t8
        S.op("dve", lambda e, g=g, dst=dst: e.tensor_tensor(dst[:], lgE[:, :, g, :], bc(gmask[:, :, g:g + 1], [128, NT, 8]), ALU.mult),
             r=[lg, gmask], w=[dst])
        if g > 0:
            S.op("dve", lambda e: e.tensor_tensor(ein[:], ein[:], t8[:], ALU.add), r=[ein, t8], w=[ein])
    S.op("dve", lambda e: e.reduce_max(sc[:, 3, :], ein[:], AX.X), r=[ein], w=[sc])
    S.op("dve", lambda e: e.tensor_tensor(m1k[:], ein[:], bc(sc[:, 3, :].unsqueeze(2), [128, NT, 8]), ALU.is_equal), r=[ein, sc], w=[m1k])
    first_only(m1k, 8)
    S.op("dve", lambda e: e.scalar_tensor_tensor(ein2[:], m1k[:], -1e30, ein[:], ALU.mult, ALU.add), r=[m1k, ein], w=[ein2])
    S.op("dve", lambda e: e.reduce_max(sc[:, 4, :], ein2[:], AX.X), r=[ein2], w=[sc])
    S.op("dve", lambda e: e.tensor_tensor(m2k[:], ein2[:], bc(sc[:, 4, :].unsqueeze(2), [128, NT, 8]), ALU.is_equal), r=[ein2, sc], w=[m2k])
    first_only(m2k, 8)
    S.op("dve", lambda e: e.tensor_tensor(sc[:, 5, :], sc[:, 4, :], sc[:, 3, :], ALU.subtract), r=[sc], w=[sc])
    S.op("act", lambda e: e.activation(out=sc[:, 6, :], in_=sc[:, 5, :], func=AF.Exp), r=[sc], w=[sc])
    S.op("dve", lambda e: e.tensor_scalar(sc[:, 7, :], sc[:, 6, :], 1.0, None, ALU.add), r=[sc], w=[sc])
    S.op("dve", lambda e: e.reciprocal(sc[:, 8, :], sc[:, 7, :]), r=[sc], w=[sc])
    S.op("dve", lambda e: e.tensor_tensor(gw[:, 0, :], sc[:, 8, :], sc[:, 2, :], ALU.mult), r=[sc], w=[gw])
    S.op("dve", lambda e: e.tensor_tensor(gw[:, 1, :], sc[:, 2, :], gw[:, 0, :], ALU.subtract), r=[sc, gw], w=[gw])
    for (ohX, mk) in ((oh1, m1k), (oh2, m2k)):
        ohv = ohX[:].rearrange("p t (g e) -> p t g e", g=4)
        for g in range(4):
            S.op("dve", lambda e, g=g, ohv=ohv, mk=mk: e.tensor_tensor(ohv[:, :, g, :], mk[:], bc(gmask[:, :, g:g + 1], [128, NT, 8]), ALU.mult),
                 r=[gmask, mk], w=[ohX])
    S.op("dve", lambda e: e.tensor_tensor(ohs[:], oh1[:], oh2[:], ALU.add), r=[oh1, oh2], w=[ohs])
    ohsf = ohs[:].rearrange("p t e -> p (t e)")
    Pff = Pf[:].rearrange("p t e -> p (t e)")
    cnf = cntf[:].rearrange("p t e -> p (t e)")
    ncol = NT * 32
    for q in range(nq):
        c0, c1 = q * 512, min(ncol, (q + 1) * 512)
        a, b_ = pp[q % len(pp)], pcn[q % len(pcn)]
        S.op("pe", _mm(a[:, 0:c1 - c0], ustr[:], ohsf[:, c0:c1], True, True), r=[ustr, ohs], w=[a])
        S.op("pe", _mm(b_[:, 0:c1 - c0], onesb[:], ohsf[:, c0:c1], True, True), r=[onesb, ohs], w=[b_])
        S.op("dve", lambda e, a=a, c0=c0, c1=c1: e.tensor_copy(Pff[:, c0:c1], a[:, 0:c1 - c0]), r=[a], w=[Pf])
        S.op("act", lambda e, b_=b_, c0=c0, c1=c1: e.copy(cnf[:, c0:c1], b_[:, 0:c1 - c0]), r=[b_], w=[cntf])
    cur, oth = cntf, base
    src0 = cntf
    d_ = 1
    bufs = [base, tmp]
    bi = 0
    cur = cntf
    while d_ < NT:
        nxt = bufs[bi % 2]
        bi += 1
        S.op("dve", lambda e, cur=cur, nxt=nxt, d_=d_: e.tensor_copy(nxt[:, 0:d_, :], cur[:, 0:d_, :]), r=[cur], w=[nxt])
        S.op("dve", lambda e, cur=cur, nxt=nxt, d_=d_: e.tensor_tensor(nxt[:, d_:NT, :], cur[:, d_:NT, :], cur[:, 0:NT - d_, :], ALU.add),
             r=[cur], w=[nxt])
        cur = nxt
        d_ *= 2
    S.op("dve", lambda e, cur=cur: e.tensor_copy(run[:], cur[:, NT - 1, :]), r=[cur], w=[run])
    if cur is base:
        S.op("dve", lambda e: e.tensor_tensor(base[:], base[:], cntf[:], ALU.subtract), r=[base, cntf], w=[base])
    else:
        S.op("dve", lambda e, cur=cur: e.tensor_tensor(base[:], cur[:], cntf[:], ALU.subtract), r=[cur, cntf], w=[base])
    S.op("dve", lambda e: e.tensor_tensor(Pf[:], Pf[:], base[:], ALU.add), r=[Pf, base], w=[Pf])
    for kk, ohX in ((0, oh1), (1, oh2)):
        S.op("dve", lambda e, ohX=ohX: e.tensor_tensor(tmp[:], Pf[:], ohX[:], ALU.mult), r=[Pf, ohX], w=[tmp])
        S.op("dve", lambda e, kk=kk: e.reduce_sum(rk[:, kk, :], tmp[:], AX.X), r=[tmp], w=[rk])


def phase_D(K, pes, P):
    nc, S, I, Dd = K.nc, K.S, K.I, K.Dd
    NT, NBLK = K.NT, K.NBLK
    sb, ps = mk_alloc(K, pes)
    oh1, oh2, rk, run, d1i, idxw, iota, bpos, identb = (P[k] for k in ("oh1", "oh2", "rk", "run", "d1i", "idxw", "iota", "bpos", "identb"))
    ci = sb("ciD", [128, 32], I32)
    padded = sb("paddedD", [128, 32], F32)
    pend = sb("pendD", [128, 32], F32)
    pstart = sb("pstartD", [128, 32], F32)
    big = sb("bigD", [128, max(NT, NBLK), 32], F32)
    df = sb("dfD", [128, 2, NT], F32)
    be = sb("beD", [128, NBLK], F32)
    bf_ = sb("bfD", [128, 6, NBLK], F32)
    S.op("dve", lambda e: e.tensor_scalar(padded[:], run[:], float(MB - 1), None, ALU.add), r=[run], w=[padded])
    S.op("dve", lambda e: e.tensor_copy(ci[:], padded[:]), r=[padded], w=[ci])
    SH = MB.bit_length() - 1
    S.op("dve", lambda e: e.tensor_scalar(ci[:], ci[:], SH, None, ALU.arith_shift_right), r=[ci], w=[ci])
    S.op("dve", lambda e: e.tensor_scalar(ci[:], ci[:], SH, None, ALU.logical_shift_left), r=[ci], w=[ci])
    S.op("dve", lambda e: e.tensor_copy(padded[:], ci[:]), r=[ci], w=[padded])
    S.op("dve", lambda e: e.tensor_copy(pend[:, 0:1], padded[:, 0:1]), r=[padded], w=[pend])
    for e_ in range(1, 32):
        S.op("dve", lambda e, e_=e_: e.tensor_tensor(pend[:, e_:e_ + 1], pend[:, e_ - 1:e_], padded[:, e_:e_ + 1], ALU.add),
             r=[pend, padded], w=[pend])
    S.op("dve", lambda e: e.tensor_tensor(pstart[:], pend[:], padded[:], ALU.subtract), r=[pend, padded], w=[pstart])
    for kk, ohX in ((0, oh1), (1, oh2)):
        S.op("dve", lambda e, ohX=ohX: e.tensor_tensor(big[:, 0:NT, :], ohX[:], bc(pstart[:].unsqueeze(1), [128, NT, 32]), ALU.mult),
             r=[ohX, pstart], w=[big])
        S.op("dve", lambda e, kk=kk: e.reduce_sum(df[:, kk, :], big[:, 0:NT, :], AX.X), r=[big], w=[df])
    S.op("dve", lambda e: e.tensor_tensor(df[:], df[:], rk[:], ALU.add), r=[df, rk], w=[df])
    S.op("dve", lambda e: e.tensor_copy(d1i[:], df[:]), r=[df], w=[d1i])
    S.op("dve", lambda e: e.tensor_tensor(big[:, 0:NBLK, :], bc(pend[:].unsqueeze(1), [128, NBLK, 32]),
         bc(bpos[:].unsqueeze(2), [128, NBLK, 32]), ALU.is_le), r=[pend, bpos], w=[big])
    S.op("dve", lambda e: e.reduce_sum(be[:], big[:, 0:NBLK, :], AX.X), r=[big], w=[be])
    S.op("dve", lambda e: e.tensor_scalar(be[:], be[:], 31.0, None, ALU.min), r=[be], w=[be])
    for q in range(6):
        if q < 2:
            S.op("dve", lambda e, q=q: e.tensor_scalar(bf_[:, q, :], be[:], 256.0, float(q), ALU.mult, ALU.add), r=[be], w=[bf_])
            S.op("dve", lambda e, q=q: e.scalar_tensor_tensor(bf_[:, q, :], bc(iota[:, 0:1], [128, NBLK]), 2.0, bf_[:, q, :], ALU.mult, ALU.add),
                 r=[iota, bf_], w=[bf_])
        else:
            S.op("dve", lambda e, q=q: e.tensor_scalar(bf_[:, q, :], be[:], 512.0, float((q - 2) * 128), ALU.mult, ALU.add), r=[be], w=[bf_])
            S.op("dve", lambda e, q=q: e.tensor_tensor(bf_[:, q, :], bf_[:, q, :], bc(iota[:, 0:1], [128, NBLK]), ALU.add),
                 r=[iota, bf_], w=[bf_])
    chg = sb("chgD", [128, NBLK], F32)
    HB = NBLK // 2
    S.op("dve", lambda e: e.memset(chg[:], 1.0), w=[chg])
    S.op("dve", lambda e: e.tensor_tensor(chg[:, 1:HB], be[:, 1:HB], be[:, 0:HB - 1], ALU.not_equal), r=[be], w=[chg])
    S.op("dve", lambda e: e.tensor_tensor(chg[:, HB + 1:NBLK], be[:, HB + 1:NBLK], be[:, HB:NBLK - 1], ALU.not_equal), r=[be], w=[chg])
    S.op("dve", lambda e: e.tensor_scalar(chg[:], chg[:], -float(2 ** 30), float(2 ** 30), ALU.mult, ALU.add), r=[chg], w=[chg])
    S.op("dve", lambda e: e.tensor_tensor(bf_[:], bf_[:], bc(chg[:].unsqueeze(1), [128, 6, NBLK]), ALU.add), r=[bf_, chg], w=[bf_])
    S.op("dve", lambda e: e.tensor_copy(idxw[:], bf_[:]), r=[bf_], w=[idxw])
    with contextlib.ExitStack() as ses:
        sbs, _ = mk_alloc(K, ses)
        TG = 8
        u2g = [sbs(f"u2gD{i}", [128, TG, D], BF16, dma=True) for i in range(NT // TG)]
        for gi_, u_ in enumerate(u2g):
            S.dma("sp", lambda e, u_=u_, gi_=gi_: e.dma_start(out=u_[:],
                  in_=Dd["U2"][gi_ * TG * 128:(gi_ + 1) * TG * 128, :].rearrange("(j p) d -> p j d", p=128)), u_, w=[u_])
        for t in range(NT):
            u_ = u2g[t // TG]
            for kk in range(2):
                S.dma("pool", lambda e, u_=u_, t=t, kk=kk: e.indirect_dma_start(out=Dd["XS"],
                      out_offset=bass.IndirectOffsetOnAxis(ap=d1i[:, kk, t:t + 1], axis=0), in_=u_[:, t % TG, :], in_offset=None),
                      u_, r=[u_, d1i])
        S.barrier()
    breg = nc.gpsimd.to_reg(NEXP * DEXP - 1)

    def streamD(sid, blocks):
        def T(n, shape, dt, dma=False):
            return sb(f"{n}X{sid}", shape, dt, dma=dma)
        wg_ = T("wg", [128, 8, 512], BF16, True)
        wu_ = T("wu", [128, 8, 512], BF16, True)
        wd_ = T("wd", [128, 4, D], BF16, True)
        NS = MB // 128
        xb_ = T("xb", [128, NS, D], BF16, True)
        xT = T("xT", [128, 8, MB], BF16)
        sg = [T(f"sg{i}", [128, MB], F32) for i in range(2)]
        hT = T("hT", [128, 4, MB], BF16)
        yt = [T(f"yt{i}", [128, D], BF16, True) for i in range(2)]
        bk = [ps(f"bkX{sid}_{i}", [128, 512]) for i in range(4)]
        pr = bk[0]
        prb = pr.t[:].bitcast(BF16).rearrange("p (c t) -> p c t", c=8)
        nyt = 0
        for b in blocks:
            for (wt_, src) in ((wg_, I["expert_w_gate"]), (wu_, I["expert_w_up"])):
                for q in range(2):
                    S.dma("pool", lambda e, wt_=wt_, src=src, q=q: e.indirect_dma_start(
                        out=wt_[:, 4 * q:4 * q + 4, :].rearrange("p j f -> p (j f)"), out_offset=None, in_=src,
                        in_offset=bass.IndirectOffsetOnAxis(ap=idxw[:, q, b:b + 1], axis=0),
                        bounds_check=breg, oob_is_err=False), wt_, r=[idxw], w=[wt_])
            for fc in range(4):
                S.dma("pool", lambda e, fc=fc: e.indirect_dma_start(out=wd_[:, fc, :], out_offset=None, in_=I["expert_w_down"],
                      in_offset=bass.IndirectOffsetOnAxis(ap=idxw[:, 2 + fc, b:b + 1], axis=0),
                      bounds_check=breg, oob_is_err=False), wd_, r=[idxw], w=[wd_])
            S.dma("sp", lambda e: e.dma_start(out=xb_[:], in_=Dd["XS"][b * MB:(b + 1) * MB, :].rearrange("(s p) d -> p s d", p=128)),
                  xb_, w=[xb_])
            yield
            for s_ in range(NS):
                for c in range(8):
                    S.op("pe", lambda e, c=c, s_=s_: e.transpose(prb[:, c, :], xb_[:, s_, c * 128:(c + 1) * 128], identb[:]),
                         r=[xb_, identb], w=[pr])
                if s_ % 2 == 0:
                    S.op("act", lambda e, s_=s_: e.copy(xT[:, :, s_ * 128:(s_ + 1) * 128], prb), r=[pr], w=[xT])
                else:
                    S.op("dve", lambda e, s_=s_: e.tensor_copy(xT[:, :, s_ * 128:(s_ + 1) * 128], prb), r=[pr], w=[xT])
                yield
            for fc in range(4):
                sg_ = sg[fc % 2]
                pg_, pu_ = (bk[1], bk[2]) if fc % 2 == 0 else (bk[3], bk[0])
                for j in range(8):
                    S.op("pe", _mm(pg_[:, 0:MB], wg_[:, j, fc * 128:(fc + 1) * 128], xT[:, j, :], j == 0, j == 7), r=[wg_, xT], w=[pg_])
                for j in range(8):
                    S.op("pe", _mm(pu_[:, 0:MB], wu_[:, j, fc * 128:(fc + 1) * 128], xT[:, j, :], j == 0, j == 7), r=[wu_, xT], w=[pu_])
                S.op("act", lambda e, sg_=sg_, pg_=pg_: e.activation(out=sg_[:], in_=pg_[:, 0:MB], func=AF.Silu), r=[pg_], w=[sg_])
                S.op("dve", lambda e, sg_=sg_, fc=fc, pu_=pu_: e.tensor_tensor(hT[:, fc, :], pu_[:, 0:MB], sg_[:], ALU.mult), r=[pu_, sg_], w=[hT])
                yield
            for s_ in range(NS):
                y_ = yt[nyt % 2]
                nyt += 1
                for h in range(2):
                    py_ = bk[(2 * s_ + h + 1) % 4]
                    for fc in range(4):
                        S.op("pe", _mm(py_[:], hT[:, fc, s_ * 128:(s_ + 1) * 128], wd_[:, fc, h * 512:(h + 1) * 512], fc == 0, fc == 3),
                             r=[hT, wd_], w=[py_])
                    if h == 0:
                        S.op("act", lambda e, y_=y_, py_=py_: e.copy(y_[:, 0:512], py_[:]), r=[py_], w=[y_])
                    else:
                        S.op("dve", lambda e, y_=y_, py_=py_: e.tensor_copy(y_[:, 512:1024], py_[:]), r=[py_], w=[y_])
                r0 = b * MB + s_ * 128
                S.dma("sp", lambda e, y_=y_, r0=r0: e.dma_start(out=Dd["Y"][r0:r0 + 128, :], in_=y_[:]), y_, r=[y_])
                yield

    gens = [streamD(0, list(range(0, NBLK // 2))), streamD(1, list(range(NBLK // 2, NBLK)))]
    for _ in range(6):
        next(gens[0])
    run_streams(gens)


def phase_E(K, pes, P):
    nc, S, I, Dd = K.nc, K.S, K.I, K.Dd
    NT = K.NT
    sb, ps = mk_alloc(K, pes)
    gw, d1i, neghalf, identb = P["gw"], P["d1i"], P["neghalf"], P["identb"]
    gple = load_colvec(K, sb, "gple", I["norm_ple_g"], 8)
    wst = [sb(f"wstE{i}", [128, 2048], F32, dma=True) for i in range(2)]
    wpg = load_weight_bf(K, sb, "wpg", I["w_ple_gate"], D, D, gvec=gple, stage=wst)
    wpp = load_weight_bf(K, sb, "wpp", I["w_ple_proj"], 256, D)
    gfin = load_rowbc(K, sb, "gfin", I["final_norm_g"], D)
    junk = sb("junkE", [128, D], BF16)

    def streamE(sid, tiles):
        def T(n, shape, dt, dma=False):
            return sb(f"{n}E{sid}", shape, dt, dma=dma)
        hL = [T(f"h1t{i}", [128, D], F32, True) for i in range(2)]
        yaL = [T(f"ya{i}", [128, D], BF16, True) for i in range(2)]
        ybL = [T(f"yb{i}", [128, D], BF16, True) for i in range(2)]
        pL_ = [T(f"pt{i}", [128, 256], F32, True) for i in range(2)]
        o_ = T("ot", [128, D], F32, True)
        h2 = T("h2", [128, D], F32)
        ss = T("ss", [128, 1], F32)
        vv = T("vv", [128, 1], F32)
        rstd = T("rstd", [128, 1], F32)
        u3 = T("u3", [128, D], BF16)
        u3T = T("u3T", [128, 8, 128], BF16)
        pb_ = T("pb", [128, 256], BF16)
        pT_ = T("pT", [128, 2, 128], BF16)
        pgt = T("pgt", [128, D], F32)
        pA_ = ps(f"pzE{sid}", [128, 512])
        pr = ps(f"prE{sid}", [128, 512])
        pz = [pA_, pA_]
        pq = pr
        prb = pr.t[:].bitcast(BF16).rearrange("p (c t) -> p c t", c=8)
        def loads(i):
            t = tiles[i]
            h_, ya_, yb_, p_ = hL[i % 2], yaL[i % 2], ybL[i % 2], pL_[i % 2]
            S.dma("sp", lambda e: e.dma_start(out=h_[:], in_=Dd["H1"][t * 128:(t + 1) * 128, :]), h_, w=[h_])
            S.dma("sp", lambda e: e.dma_start(out=p_[:], in_=I["p"][t * 128:(t + 1) * 128, :]), p_, w=[p_])
            for kk, y_ in ((0, ya_), (1, yb_)):
                S.dma("pool", lambda e, y_=y_, kk=kk: e.indirect_dma_start(out=y_[:], out_offset=None, in_=Dd["Y"],
                      in_offset=bass.IndirectOffsetOnAxis(ap=d1i[:, kk, t:t + 1], axis=0)), y_, r=[d1i], w=[y_])

        loads(0)
        for i, t in enumerate(tiles):
            h_, ya_, yb_, p_ = hL[i % 2], yaL[i % 2], ybL[i % 2], pL_[i % 2]
            if i + 1 < len(tiles):
                loads(i + 1)
            yield
            S.op("dve", lambda e: e.scalar_tensor_tensor(h2[:], ya_[:], gw[:, 0, t:t + 1], h_[:], ALU.mult, ALU.add),
                 r=[ya_, gw, h_], w=[h2])
            S.op("dve", lambda e: e.scalar_tensor_tensor(h2[:], yb_[:], gw[:, 1, t:t + 1], h2[:], ALU.mult, ALU.add),
                 r=[yb_, gw, h2], w=[h2])
            S.op("act", lambda e: e.activation(out=junk[:], in_=h2[:], func=AF.Square, accum_out=ss[:, 0:1]), r=[h2], w=[junk, ss])
            rstd_from_ss(K, ss, rstd, vv, neghalf)
            yield
            S.op("act", lambda e: e.activation(out=u3[:], in_=h2[:], func=AF.Copy, scale=rstd[:, 0:1]), r=[h2, rstd], w=[u3])
            for c in range(8):
                S.op("pe", lambda e, c=c: e.transpose(prb[:, c, :], u3[:, c * 128:(c + 1) * 128], identb[:]), r=[u3, identb], w=[pr])
            S.op("act", lambda e: e.copy(u3T[:], prb), r=[pr], w=[u3T])
            yield
            S.op("dve", lambda e: e.tensor_copy(pb_[:], p_[:]), r=[p_], w=[pb_])
            for c in range(2):
                S.op("pe", lambda e, c=c: e.transpose(prb[:, c, :], pb_[:, c * 128:(c + 1) * 128], identb[:]), r=[pb_, identb], w=[pr])
            S.op("act", lambda e: e.copy(pT_[:], prb[:, 0:2, :]), r=[pr], w=[pT_])
            yield
            for h in range(2):
                sl = slice(h * 512, (h + 1) * 512)
                for c in range(8):
                    S.op("pe", _mm(pz[h][:], u3T[:, c, :], wpg[:, c, sl], c == 0, c == 7), r=[u3T, wpg], w=[pz[h]])
                S.op("act", lambda e, h=h, sl=sl: e.activation(out=pgt[:, sl], in_=pz[h][:], func=AF.Sigmoid), r=[pz[h]], w=[pgt])
                for c in range(2):
                    S.op("pe", _mm(pq[:], pT_[:, c, :], wpp[:, c, sl], c == 0, c == 1), r=[pT_, wpp], w=[pq])
                S.op("dve", lambda e, h=h, sl=sl: e.tensor_tensor(pgt[:, sl], pq[:], pgt[:, sl], ALU.mult), r=[pq, pgt], w=[pgt])
                yield
            S.op("dve", lambda e: e.tensor_tensor(h2[:], h2[:], pgt[:], ALU.add), r=[h2, pgt], w=[h2])
            S.op("act", lambda e: e.activation(out=junk[:], in_=h2[:], func=AF.Square, accum_out=ss[:, 0:1]), r=[h2], w=[junk, ss])
            rstd_from_ss(K, ss, rstd, vv, neghalf)
            yield
            S.op("dve", lambda e: e.scalar_tensor_tensor(o_[:], h2[:], rstd[:, 0:1], gfin[:], ALU.mult, ALU.mult),
                 r=[h2, rstd, gfin], w=[o_])
            S.dma("sp", lambda e: e.dma_start(out=K.out[t * 128:(t + 1) * 128, :], in_=o_[:]), o_, r=[o_])
            yield

    gens = [streamE(i, list(range(i, NT, 4))) for i in range(4)]
    for i in range(3):
        for _ in range(2 * (3 - i)):
            next(gens[i])
    run_streams(gens)
    S.barrier()
```

```python
import contextlib
import numpy as np
import concourse.bass as bass
import concourse.mybir as mybir
from concourse.bass_utils import run_bass_kernel_spmd

F32 = mybir.dt.float32
BF16 = mybir.dt.bfloat16
I32 = mybir.dt.int32
AF = mybir.ActivationFunctionType
ALU = mybir.AluOpType
AX = mybir.AxisListType

D = 1024
SEQ = 2048
NIN = 6672
EPS = 1e-6
OFF_XBC, OFF_DT, OFF_GLU, OFF_GATE = 1024, 2560, 2576, 4624
NEXP = 32
DEXP = 512
MB = 512
ROT = 30000
B_OFFSET = 7
C1_OFFSET = 5
D_OFFSET = 6


class Buf:
    _n = 0

    def __init__(self, name, t=None, sem=None):
        Buf._n += 1
        self.uid = Buf._n
        self.name = name
        self.t = t
        self.lw = None
        self.rd = {}
        self.sem = sem
        self.dn = 0
        self.dw = 0

    def __getitem__(self, k):
        return self.t[k]


class Sched:
    CE = ("pe", "act", "dve", "pool")

    def __init__(self, nc, es):
        self.nc = nc
        self.es = es
        self.e = {"pe": nc.tensor, "act": nc.scalar, "dve": nc.vector, "pool": nc.gpsimd, "sp": nc.sync}
        self.cnt = {k: 0 for k in self.CE}
        self.sems = {k: [] for k in self.CE}
        self.waited = {}
        self.dbufs = []
        self.nsem = 0
        self.sempool = []

    def newsem(self, name):
        self.nsem += 1
        return self.es.enter_context(self.nc.semaphore(f"{name}_{self.nsem}"))

    def buf(self, name, t=None, dma=False):
        b = Buf(name, t, None)
        if dma == "sw":
            b.sem = self.newsem("swdma")
            b.fresh = True
            self.dbufs.append(b)
        elif dma:
            if self.sempool:
                b.sem, b.dn = self.sempool.pop()
            else:
                b.sem = self.newsem("dma")
            self.dbufs.append(b)
        return b

    def release(self, bufs):
        for b in bufs:
            if b.sem is not None and b in self.dbufs:
                self.dbufs.remove(b)
                if not getattr(b, "fresh", False):
                    self.sempool.append((b.sem, b.dn))
                b.sem = None

    def _semval(self, eng, seq):
        i = (seq - 1) // ROT
        while len(self.sems[eng]) <= i:
            self.sems[eng].append(self.newsem(eng))
        return self.sems[eng][i], (seq - 1) % ROT + 1

    def _emit_waits(self, waiter, cdeps, ddeps):
        w = self.e[waiter]
        for src, seq in cdeps.items():
            if waiter == "pe" and src == "pe":
                continue
            key = (waiter, src)
            if self.waited.get(key, 0) >= seq:
                continue
            self.waited[key] = seq
            sem, val = self._semval(src, seq)
            w.wait_ge(sem, val)
        for b, n in ddeps.items():
            key = (waiter, "d", b.uid)
            if self.waited.get(key, 0) >= n:
                continue
            self.waited[key] = n
            w.wait_ge(b.sem, 16 * n)

    @staticmethod
    def _deps(r, w):
        cd, dd = {}, {}

        def addc(x):
            if x is not None and cd.get(x[0], 0) < x[1]:
                cd[x[0]] = x[1]

        for b in r:
            addc(b.lw)
            if b.dw:
                dd[b] = max(dd.get(b, 0), b.dw)
        for b in w:
            addc(b.lw)
            for e_, s_ in b.rd.items():
                addc((e_, s_))
            if b.dn:
                dd[b] = max(dd.get(b, 0), b.dn)
        return cd, dd

    def op(self, eng, fn, r=(), w=()):
        cd, dd = self._deps(r, w)
        self._emit_waits(eng, cd, dd)
        ins = fn(self.e[eng])
        self.cnt[eng] += 1
        seq = self.cnt[eng]
        sem, _ = self._semval(eng, seq)
        ins.then_inc(sem, 1)
        for b in r:
            b.rd[eng] = seq
        for b in w:
            b.lw = (eng, seq)
            b.rd = {}

    def dma(self, q, fn, prim, r=(), w=()):
        cd, dd = self._deps(r, w)
        self._emit_waits(q, cd, dd)
        ins = fn(self.e[q])
        ins.then_inc(prim.sem, 16)
        prim.dn += 1
        if prim in w:
            prim.dw = prim.dn

    def barrier(self):
        for waiter in ("pe", "act", "dve", "pool", "sp"):
            cd = {k: self.cnt[k] for k in self.CE if self.cnt[k] > 0}
            dd = {b: b.dn for b in self.dbufs if b.dn > 0}
            w = self.e[waiter]
            for src, seq in cd.items():
                key = (waiter, src)
                if self.waited.get(key, 0) >= seq:
                    continue
                self.waited[key] = seq
                sem, val = self._semval(src, seq)
                w.wait_ge(sem, val)
            for b, n in dd.items():
                key = (waiter, "d", b.uid)
                if self.waited.get(key, 0) >= n:
                    continue
                self.waited[key] = n
                w.wait_ge(b.sem, 16 * n)


class Ctx:
    pass


def _mm(out, lhsT, rhs, start, stop, skip=False):
    if skip:
        return lambda e: e.matmul(out, lhsT, rhs, start=start, stop=stop, skip_group_check=True)
    return lambda e: e.matmul(out, lhsT, rhs, start=start, stop=stop)


def build(nseq, debug=False, upto="E"):
    T = nseq * SEQ
    NT = T // 128
    NG = T // 512
    NBLK = (2 * T) // MB + NEXP
    NROWS = NBLK * MB
    nc = bass.Bass("TRN2", target_bir_lowering=False)
    K = Ctx()
    K.nc, K.T, K.NT, K.NG, K.NBLK, K.NROWS, K.nseq = nc, T, NT, NG, NBLK, NROWS, nseq

    def din(name, shape, dt=F32):
        return nc.dram_tensor(name, list(shape), dt, kind="ExternalInput").ap()

    def dscr(name, shape, dt):
        return nc.dram_tensor(name, list(shape), dt, kind="ExternalOutput" if debug else "Internal").ap()

    I = {}
    I["x"] = din("x", [T, D])
    I["p"] = din("p", [T, 256])
    I["norm_mix_g"] = din("norm_mix_g", [D])
    I["w_in"] = din("w_in", [D, NIN])
    I["ssd_conv_w"] = din("ssd_conv_w", [4, 1536])
    I["ssd_conv_b"] = din("ssd_conv_b", [1536])
    I["ssd_dt_bias"] = din("ssd_dt_bias", [16])
    I["ssd_a_log"] = din("ssd_a_log", [16])
    I["ssd_d"] = din("ssd_d", [16])
    I["ssd_norm_g"] = din("ssd_norm_g", [D])
    I["w_ssd_out"] = din("w_ssd_out", [D, D])
    I["conf_dw_w"] = din("conf_dw_w", [31, D])
    I["conf_dw_b"] = din("conf_dw_b", [D])
    I["conf_ln_g"] = din("conf_ln_g", [D])
    I["conf_ln_b"] = din("conf_ln_b", [D])
    I["w_conf_out"] = din("w_conf_out", [D, D])
    I["w_o"] = din("w_o", [D, D])
    I["norm_ffn_g"] = din("norm_ffn_g", [D])
    I["router_w"] = din("router_w", [D, 36])
    I["router_b"] = din("router_b", [36])
    I["expert_w_gate"] = din("expert_w_gate", [NEXP * D * DEXP // 2048, 2048])
    I["expert_w_up"] = din("expert_w_up", [NEXP * D * DEXP // 2048, 2048])
    I["expert_w_down"] = din("expert_w_down", [NEXP * DEXP, D])
    I["norm_ple_g"] = din("norm_ple_g", [D])
    I["w_ple_gate"] = din("w_ple_gate", [D, D])
    I["w_ple_proj"] = din("w_ple_proj", [256, D])
    I["final_norm_g"] = din("final_norm_g", [D])
    I["c_ident"] = din("c_ident", [128, 128])
    I["c_trile"] = din("c_trile", [128, 128])
    I["c_ustr"] = din("c_ustr", [128, 128])
    I["c_e3"] = din("c_e3", [16, 16 * 128])
    I["c_iota"] = din("c_iota", [128, 1])
    I["c_bpos"] = din("c_bpos", [128, NBLK])
    K.I = I
    K.out = nc.dram_tensor("out", [T, D], F32, kind="ExternalOutput").ap()
    Dd = {}
    Dd["SZ"] = dscr("SZ", [T, D], BF16)
    Dd["XBCT"] = dscr("XBCT", [1536, T], BF16)
    Dd["GLUT"] = dscr("GLUT", [D, T], BF16)
    Dd["GATET"] = dscr("GATET", [2 * D, T], BF16)
    Dd["DTR"] = dscr("DTR", [T, 16], F32)
    Dd["GS1T"] = dscr("GS1T", [D, T], BF16)
    Dd["GST"] = dscr("GST", [D, T], BF16)
    Dd["H1"] = dscr("H1", [T, D], F32)
    Dd["U2"] = dscr("U2", [T, D], BF16)
    Dd["XS"] = dscr("XS", [NROWS, D], BF16)
    Dd["Y"] = dscr("Y", [NROWS, D], BF16)
    K.Dd = Dd
    K.debug = debug

    with contextlib.ExitStack() as es:
        S = Sched(nc, es)
        K.S = S
        with contextlib.ExitStack() as pes:
            phase_A(K, pes)
            S.barrier()
        if upto >= "B":
            with contextlib.ExitStack() as pes:
                phase_B(K, pes)
                S.barrier()
        if upto >= "C":
            with contextlib.ExitStack() as pes2:
                phase_CDE(K, pes2, upto)
                S.barrier()
    return nc


def mk_alloc(K, pes):
    nc, S = K.nc, K.S

    K.uid = getattr(K, "uid", 0) + 1
    pfx = f"P{K.uid}_"
    mine = []
    pes.callback(lambda: S.release(mine))

    def sb(name, shape, dt, dma=False):
        t = pes.enter_context(nc.sbuf_tensor(pfx + name, list(shape), dt))
        b = S.buf(pfx + name, t, dma=dma)
        mine.append(b)
        return b

    def ps(name, shape, dt=F32):
        t = pes.enter_context(nc.psum_tensor(pfx + name, list(shape), dt))
        return S.buf(pfx + name, t)

    return sb, ps


def load_consts(K, sb, names):
    nc, S, I = K.nc, K.S, K.I
    C = {}
    for nm in names:
        if nm == "ident_bf":
            C[nm] = sb("c_identb", [128, 128], BF16, dma="sw")
            S.dma("pool", lambda e, b=C[nm]: e.dma_start(out=b[:], in_=I["c_ident"]), C[nm], w=[C[nm]])
        elif nm == "ident_f":
            C[nm] = sb("c_identf", [128, 128], F32, dma=True)
            S.dma("sp", lambda e, b=C[nm]: e.dma_start(out=b[:], in_=I["c_ident"]), C[nm], w=[C[nm]])
        elif nm == "trile_f":
            C[nm] = sb("c_trilef", [128, 128], F32, dma=True)
            S.dma("sp", lambda e, b=C[nm]: e.dma_start(out=b[:], in_=I["c_trile"]), C[nm], w=[C[nm]])
        elif nm == "ustr_bf":
            C[nm] = sb("c_ustrb", [128, 128], BF16, dma="sw")
            S.dma("pool", lambda e, b=C[nm]: e.dma_start(out=b[:], in_=I["c_ustr"]), C[nm], w=[C[nm]])
        elif nm == "e3":
            C[nm] = sb("c_e3s", [16, 2048], F32, dma=True)
            S.dma("sp", lambda e, b=C[nm]: e.dma_start(out=b[:], in_=I["c_e3"]), C[nm], w=[C[nm]])
        elif nm == "iota":
            C[nm] = sb("c_iotas", [128, 1], F32, dma=True)
            S.dma("sp", lambda e, b=C[nm]: e.dma_start(out=b[:], in_=I["c_iota"]), C[nm], w=[C[nm]])
        elif nm == "bpos":
            C[nm] = sb("c_bposs", [128, K.NBLK], F32, dma=True)
            S.dma("sp", lambda e, b=C[nm]: e.dma_start(out=b[:], in_=I["c_bpos"]), C[nm], w=[C[nm]])
        elif nm == "neghalf":
            C[nm] = sb("c_neghalf", [128, 512], F32)
            S.op("pool", lambda e, b=C[nm]: e.memset(b[:], -0.5), w=[C[nm]])
        elif nm == "ones_bf":
            C[nm] = sb("c_onesb", [128, 128], BF16)
            S.op("pool", lambda e, b=C[nm]: e.memset(b[:], 1.0), w=[C[nm]])
        elif nm == "ones_f":
            C[nm] = sb("c_onesf", [128, 128], F32)
            S.op("pool", lambda e, b=C[nm]: e.memset(b[:], 1.0), w=[C[nm]])
        elif nm == "onesdiv_bf":
            C[nm] = sb("c_onesdiv", [128, 128], BF16)
            S.op("pool", lambda e, b=C[nm]: e.memset(b[:], 1.0 / 1024.0), w=[C[nm]])
    return C


def load_colvec(K, sb, name, src, nchunk, perm=False):
    nc, S = K.nc, K.S
    b = sb(name, [128, nchunk], F32, dma=True)
    if perm:
        S.dma("sp", lambda e: e.dma_start(out=b[:], in_=src.rearrange("(p c) -> p c", c=nchunk)), b, w=[b])
    else:
        with nc.allow_non_contiguous_dma(reason="small column vector"):
            S.dma("sp", lambda e: e.dma_start(out=b[:], in_=src.rearrange("(c p) -> p c", p=128)), b, w=[b])
    return b


def load_rowbc(K, sb, name, src, n):
    S = K.S
    b = sb(name, [128, n], F32, dma=True)
    S.dma("sp", lambda e: e.dma_start(out=b[:], in_=src.unsqueeze(0).partition_broadcast(128)), b, w=[b])
    return b


def load_weight_bf(K, sb, name, src, kin, nout, gvec=None, stage=None, perm=False, wb=None):
    nc, S = K.nc, K.S
    kc = kin // 128
    if wb is None:
        wb = sb(name, [128, kc, nout], BF16, dma=("sw" if gvec is None else False))
    if gvec is None:
        for c in range(kc):
            if perm:
                raise NotImplementedError
            S.dma("pool", lambda e, c=c: e.dma_start(out=wb[:, c, :], in_=src[c * 128:(c + 1) * 128, :]), wb, w=[wb])
        return wb
    i = 0
    for c in range(kc):
        for n0 in range(0, nout, 2048):
            n1 = min(nout, n0 + 2048)
            st = stage[i % len(stage)]
            if perm:
                srcap = src.rearrange("(p c) n -> p c n", c=kc)[:, c, n0:n1]
            else:
                srcap = src[c * 128:(c + 1) * 128, n0:n1]
            S.dma("sp", lambda e, st=st, srcap=srcap, n=n1 - n0: e.dma_start(out=st[:, 0:n], in_=srcap), st, w=[st])
            eng = "dve" if i % 2 == 0 else "act"
            if eng == "dve":
                S.op("dve", lambda e, st=st, c=c, n0=n0, n1=n1: e.tensor_scalar(
                    wb[:, c, n0:n1], st[:, 0:n1 - n0], gvec[:, c:c + 1], None, ALU.mult), r=[st, gvec], w=[wb])
            else:
                S.op("act", lambda e, st=st, c=c, n0=n0, n1=n1: e.activation(
                    out=wb[:, c, n0:n1], in_=st[:, 0:n1 - n0], func=AF.Copy, scale=gvec[:, c:c + 1]), r=[st, gvec], w=[wb])
            i += 1
    return wb


def rstd_from_ss(K, ss, rstd, v, neghalf, n=1024.0):
    S = K.S
    S.op("dve", lambda e: e.tensor_scalar(v[:, 0:1], ss[:, 0:1], 1.0 / n, EPS, ALU.mult, ALU.add), r=[ss], w=[v])
    S.op("pool", lambda e: e.tensor_tensor(rstd[:, 0:1], v[:, 0:1], neghalf[:, 0:1], ALU.pow), r=[v, neghalf], w=[rstd])


def phase_A(K, pes):
    nc, S, I, Dd = K.nc, K.S, K.I, K.Dd
    sb, ps = mk_alloc(K, pes)
    C = load_consts(K, sb, ["ident_bf", "neghalf"])
    gm = load_colvec(K, sb, "gm", I["norm_mix_g"], 8)
    winb = sb("winb", [128, 8, NIN], BF16)
    junk = sb("junkA", [128, D], BF16)
    with contextlib.ExitStack() as tmpes:
        sbt, _ = mk_alloc(K, tmpes)
        wst = [sbt(f"wst{i}", [128, 2048], F32, dma=True) for i in range(2)]
        load_weight_bf(K, sb, "winb", I["w_in"], D, NIN, gvec=gm, stage=wst, wb=winb)
        S.barrier()
    ident = C["ident_bf"]
    zt = sb("zeroA", [128, D], BF16, dma=True)
    S.op("pool", lambda e: e.memset(zt[:], 0.0), w=[zt])
    nzc = K.NROWS // 128
    zper = -(-nzc // K.NG)

    def zero_fill(g):
        for i in range(zper):
            ci = g * zper + i
            if ci < nzc:
                S.dma("sp", lambda e, ci=ci: e.dma_start(out=Dd["XS"][ci * 128:(ci + 1) * 128, :], in_=zt[:]), zt, r=[zt])

    def streamA(sid, groups):
        def T(n, shape, dt, dma=False):
            return sb(f"{n}A{sid}", shape, dt, dma=dma)
        xt = [T(f"xt{i}", [128, D], F32, True) for i in range(2)]
        ss = [T(f"ss{i}", [128, 1], F32) for i in range(2)]
        vv = [T(f"vv{i}", [128, 1], F32) for i in range(2)]
        rstd = [T(f"rstd{i}", [128, 1], F32) for i in range(2)]
        ub = [T(f"ub{i}", [128, D], BF16) for i in range(8)]
        u = T("uT", [128, 8, 512], BF16)
        szt = [T(f"szt{i}", [128, D], BF16, True) for i in range(2)]
        dtt = [T(f"dtt{i}", [128, 16], F32, True) for i in range(2)]
        stg = [T(f"stg{i}", [128, 4, 512], BF16, True) for i in range(2)]
        sgb = [T(f"sgb{i}", [128, 512], BF16) for i in range(2)]
        pT = ps(f"pTA{sid}", [128, 8, 128], BF16)
        pz = ps(f"pzA{sid}", [128, 512])
        pf = [ps(f"pfA{sid}_{i}", [128, 512]) for i in range(2)]
        nf = 0
        nst = 0
        def emit_norm(gidx, g, j):
            t = g * 4 + j
            x_ = xt[t % 2]
            S.dma("sp", lambda e: e.dma_start(out=x_[:], in_=I["x"][t * 128:(t + 1) * 128, :]), x_, w=[x_])
            s_, v_, r_, ub_ = ss[t % 2], vv[t % 2], rstd[t % 2], ub[(gidx % 2) * 4 + j]
            S.op("act", lambda e: e.activation(out=junk[:], in_=x_[:], func=AF.Square, accum_out=s_[:, 0:1]),
                 r=[x_], w=[junk, s_])
            rstd_from_ss(K, s_, r_, v_, C["neghalf"])
            S.op("dve", lambda e: e.tensor_scalar(ub_[:], x_[:], r_[:, 0:1], None, ALU.mult), r=[x_, r_], w=[ub_])

        for j in range(4):
            emit_norm(0, groups[0], j)
        for gidx, g in enumerate(groups):
            zero_fill(g)
            for j in range(4):
                t = g * 4 + j
                ub_ = ub[(gidx % 2) * 4 + j]
                for c in range(8):
                    S.op("pe", lambda e, c=c, ub_=ub_: e.transpose(pT[:, c, :], ub_[:, c * 128:(c + 1) * 128], ident[:]),
                         r=[ub_, ident], w=[pT])
                S.op("act", lambda e, j=j: e.copy(u[:, :, j * 128:(j + 1) * 128], pT[:]), r=[pT], w=[u])
                yield
            for j in range(4):
                t = g * 4 + j
                sz_ = szt[t % 2]
                pTf = pT.t[:].bitcast(F32)
                for h in range(2):
                    pzb, pzap = (pz, pz[:]) if h == 0 else (pT, pTf)
                    for c in range(8):
                        S.op("pe", _mm(pzap, u[:, c, j * 128:(j + 1) * 128], winb[:, c, h * 512:(h + 1) * 512], c == 0, c == 7),
                             r=[u, winb], w=[pzb])
                    sgz = sgb[h]
                    S.op("act", lambda e, sgz=sgz, pzap=pzap: e.activation(out=sgz[:], in_=pzap, func=AF.Sigmoid), r=[pzb], w=[sgz])
                    S.op("dve", lambda e, h=h, sz_=sz_, sgz=sgz, pzap=pzap: e.tensor_tensor(sz_[:, h * 512:(h + 1) * 512], pzap, sgz[:], ALU.mult),
                         r=[pzb, sgz], w=[sz_])
                S.dma("sp", lambda e, sz_=sz_, t=t: e.dma_start(out=Dd["SZ"][t * 128:(t + 1) * 128, :], in_=sz_[:]), sz_, r=[sz_])
                for c in range(8):
                    S.op("pe", _mm(pz[:, 0:16], u[:, c, j * 128:(j + 1) * 128], winb[:, c, OFF_DT:OFF_DT + 16], c == 0, c == 7),
                         r=[u, winb], w=[pz])
                d_ = dtt[t % 2]
                S.op("dve", lambda e, d_=d_: e.tensor_copy(d_[:], pz[:, 0:16]), r=[pz], w=[d_])
                S.dma("sp", lambda e, d_=d_, t=t: e.dma_start(out=Dd["DTR"][t * 128:(t + 1) * 128, :], in_=d_[:]), d_, r=[d_])
                yield
            items = [("xbc", c) for c in range(12)] + [("glu", c) for c in range(8)] + [("gate", c) for c in range(16)]
            for idx, (kind, c) in enumerate(items):
                if gidx + 1 < len(groups) and idx in (4, 8, 12, 16):
                    emit_norm(gidx + 1, groups[gidx + 1], idx // 4 - 1)
                st = stg[nst % 2]
                slot = idx % 4

                def fm(col0):
                    nonlocal nf
                    pf_ = pf[nf % 2]
                    nf += 1
                    for kc in range(8):
                        S.op("pe", _mm(pf_[:], winb[:, kc, col0:col0 + 128], u[:, kc, :], kc == 0, kc == 7), r=[u, winb], w=[pf_])
                    return pf_

                if kind == "xbc":
                    pf_ = fm(OFF_XBC + c * 128)
                    if idx % 2 == 0:
                        S.op("act", lambda e, pf_=pf_, st=st, slot=slot: e.copy(st[:, slot, :], pf_[:]), r=[pf_], w=[st])
                    else:
                        S.op("dve", lambda e, pf_=pf_, st=st, slot=slot: e.tensor_copy(st[:, slot, :], pf_[:]), r=[pf_], w=[st])
                    dst, row0 = Dd["XBCT"], (c - slot) * 128
                elif kind == "glu":
                    pa = fm(OFF_GLU + c * 128)
                    pb = fm(OFF_GLU + D + c * 128)
                    sg_ = sgb[c % 2]
                    S.op("act", lambda e, pb=pb, sg_=sg_: e.activation(out=sg_[:], in_=pb[:], func=AF.Sigmoid), r=[pb], w=[sg_])
                    S.op("dve", lambda e, pa=pa, sg_=sg_, st=st, slot=slot: e.tensor_tensor(st[:, slot, :], pa[:], sg_[:], ALU.mult),
                         r=[pa, sg_], w=[st])
                    dst, row0 = Dd["GLUT"], (c - slot) * 128
                else:
                    pf_ = fm(OFF_GATE + c * 128)
                    S.op("act", lambda e, pf_=pf_, st=st, slot=slot: e.activation(out=st[:, slot, :], in_=pf_[:], func=AF.Sigmoid),
                         r=[pf_], w=[st])
                    dst, row0 = Dd["GATET"], (c - slot) * 128
                if slot == 3:
                    S.dma("sp", lambda e, st=st, dst=dst, row0=row0, g=g: e.dma_start(
                        out=dst[row0:row0 + 512, g * 512:(g + 1) * 512].rearrange("(c p) t -> p c t", p=128), in_=st[:]),
                        st, r=[st])
                    nst += 1
                yield

    gens = [streamA(0, list(range(0, K.NG, 2))), streamA(1, list(range(1, K.NG, 2)))]
    for _ in range(12):
        next(gens[0])
    run_streams(gens)


def make_consts(nblk):
    s = np.arange(128)
    c = {}
    c["c_ident"] = np.eye(128, dtype=np.float32)
    c["c_trile"] = (s[:, None] <= s[None, :]).astype(np.float32)
    c["c_ustr"] = (s[:, None] < s[None, :]).astype(np.float32)
    e3 = np.zeros((16, 16, 128), np.float32)
    for h in range(16):
        e3[h, h, :] = 1.0
    c["c_e3"] = e3.reshape(16, 2048)
    c["c_iota"] = s.astype(np.float32).reshape(128, 1)
    c["c_bpos"] = np.broadcast_to((np.arange(nblk) * float(MB)).astype(np.float32)[None, :], (128, nblk)).copy()
    return c


def make_in_map(inp, core, nseq):
    b0 = core * nseq
    T = nseq * SEQ
    nblk = (2 * T) // MB + NEXP
    m = {}
    m["x"] = np.ascontiguousarray(inp["x"][b0:b0 + nseq].reshape(T, D))
    m["p"] = np.ascontiguousarray(inp["p"][0, b0:b0 + nseq].reshape(T, 256))
    for k in ["norm_mix_g", "w_in", "ssd_conv_w", "ssd_conv_b", "ssd_dt_bias", "ssd_a_log", "ssd_d", "ssd_norm_g",
              "w_ssd_out", "conf_dw_w", "conf_dw_b", "conf_ln_g", "conf_ln_b", "w_conf_out", "w_o", "norm_ffn_g",
              "norm_ple_g", "w_ple_gate", "w_ple_proj"]:
        m[k] = np.ascontiguousarray(inp[k][0])
    m["final_norm_g"] = np.ascontiguousarray(inp["final_norm_g"])
    m["router_w"] = np.ascontiguousarray(np.concatenate([inp["router_group_w"][0], inp["router_expert_w"][0]], axis=1))
    m["router_b"] = np.ascontiguousarray(np.concatenate([inp["router_group_b"][0], inp["router_expert_b"][0]], axis=0))
    m["expert_w_gate"] = np.ascontiguousarray(inp["expert_w_gate"][0]).reshape(-1, 2048)
    m["expert_w_up"] = np.ascontiguousarray(inp["expert_w_up"][0]).reshape(-1, 2048)
    m["expert_w_down"] = np.ascontiguousarray(inp["expert_w_down"][0]).reshape(NEXP * DEXP, D)
    m.update(make_consts(nblk))
    return m


_NC_CACHE = {}


def kernel(**inputs):
    inp = {k: np.asarray(v) for k, v in inputs.items()}
    ncores = 8
    nseq = inp["x"].shape[0] // ncores
    if nseq not in _NC_CACHE:
        _NC_CACHE[nseq] = build(nseq)
    nc = _NC_CACHE[nseq]
    in_maps = [make_in_map(inp, c, nseq) for c in range(ncores)]
    res = run_bass_kernel_spmd(nc, in_maps, core_ids=list(range(ncores)))
    outs = [np.asarray(r["out"]).reshape(nseq, SEQ, D) for r in res.results]
    return np.concatenate(outs, axis=0).astype(np.float32)


def bc(ap, shape):
    return ap.to_broadcast(list(shape))


def run_streams(gens):
    gens = list(gens)
    while gens:
        for g_ in list(gens):
            try:
                next(g_)
            except StopIteration:
                gens.remove(g_)


def phase_B(K, pes):
    nc, S, I, Dd = K.nc, K.S, K.I, K.Dd
    sb, ps = mk_alloc(K, pes)
    C = load_consts(K, sb, ["ident_bf", "ident_f", "trile_f", "ones_f", "e3", "neghalf"])
    identb, identf, trile, onesf, e3 = C["ident_bf"], C["ident_f"], C["trile_f"], C["ones_f"], C["e3"]
    cw = sb("cwB", [128, 4, 12], F32, dma=True)
    with nc.allow_non_contiguous_dma(reason="small conv weights"):
        S.dma("sp", lambda e: e.dma_start(out=cw[:], in_=I["ssd_conv_w"].rearrange("k (c p) -> p k c", p=128)), cw, w=[cw])
    cbcol = load_colvec(K, sb, "cbcolB", I["ssd_conv_b"], 12)
    cbrow = sb("cbrowB", [1, 1536], BF16, dma="sw")
    S.dma("pool", lambda e: e.dma_start(out=cbrow[:], in_=I["ssd_conv_b"].unsqueeze(0)), cbrow, w=[cbrow])
    ones1 = sb("ones1B", [1, 128], BF16)
    S.op("pool", lambda e: e.memset(ones1[:], 1.0), w=[ones1])
    diag = sb("diagB", [128, 48, 128], BF16)
    for k in range(4):
        S.op("dve", lambda e, k=k: e.tensor_tensor(diag[:, k * 12:(k + 1) * 12, :], bc(identf[:].unsqueeze(1), [128, 12, 128]),
             bc(cw[:, k, :].unsqueeze(2), [128, 12, 128]), ALU.mult), r=[identf, cw], w=[diag])
    dtb = load_rowbc(K, sb, "dtbB", I["ssd_dt_bias"], 16)
    alog = load_rowbc(K, sb, "alogB", I["ssd_a_log"], 16)
    drow = load_rowbc(K, sb, "drowB", I["ssd_d"], 16)
    arow = sb("arowB", [128, 16], F32)
    S.op("act", lambda e: e.activation(out=arow[:], in_=alog[:], func=AF.Exp), r=[alog], w=[arow])
    S.op("dve", lambda e: e.tensor_scalar(arow[:], arow[:], -1.0, None, ALU.mult), r=[arow], w=[arow])
    gssd = load_colvec(K, sb, "gssdB", I["ssd_norm_g"], 8)
    wssd = sb("wssd", [128, 8, D], BF16)
    junk = sb("junkB", [128, D], BF16)
    selb = sb("selbB", [48, 2048], BF16, dma="sw")
    S.op("pool", lambda e: e.memset(selb[:], 0.0), w=[selb])
    S.dma("pool", lambda e: e.dma_start(out=selb[0:16, :], in_=I["c_e3"]), selb, w=[selb])
    S.dma("pool", lambda e: e.dma_start(out=selb[32:48, :], in_=I["c_e3"]), selb, w=[selb])
    with contextlib.ExitStack() as tmpes:
        sbt, _ = mk_alloc(K, tmpes)
        wst = [sbt(f"wstB{i}", [128, 2048], F32, dma=True) for i in range(2)]
        load_weight_bf(K, sb, "wssd", I["w_ssd_out"], D, D, gvec=gssd, stage=wst, wb=wssd)
        S.barrier()
    neghalf = C["neghalf"]

    def stream(sid, seqs):
        def T(n, shape, dt, dma=False):
            return sb(f"{n}B{sid}", shape, dt, dma=dma)
        xin = T("xin", [128, 12, 515], BF16, True)
        szt = T("szt", [128, D], BF16, True)
        gtn = [T(f"gtn{i}", [128, 512], BF16, True) for i in range(2)]
        dtr = T("dtr", [128, 4, 16], F32, True)
        BT = T("BT", [128, 2, 512], BF16)
        CT = T("CT", [128, 2, 512], BF16)
        ynT = T("ynT", [128, 8, 512], BF16)
        stg = T("stg", [128, 4, 512], BF16, True)
        xs_ = T("xs", [128, D], F32)
        bt_ = T("btok", [128, 256], BF16)
        sm = T("sm", [128, 8, 64], F32)
        acs = T("acs", [128, 128], F32)
        dApad = T("dApad", [128, 4, 48], F32)
        S.op("pool", lambda e: e.memset(dApad[:], 0.0), w=[dApad])
        aHL = T("aHL", [48, 512], BF16)
        S.op("pool", lambda e: e.memset(aHL[:], 0.0), w=[aHL])
        tH = T("tH", [48, 512], BF16)
        nHL = T("nHL", [48, 512], BF16)
        pvb = T("pvb", [128, D], F32)
        segq = [T(f"segq{i}", [128, 512], F32) for i in range(2)]
        dm = T("dm", [128, 16, 128], BF16)
        gm_ = T("gm", [128, 2, 128], BF16)
        Mt = T("Mt", [128, 16, 128], BF16)
        xdt = T("xdt", [128, 16, 64], BF16)
        xdtd = T("xdtd", [128, 16, 64], BF16)
        y1 = T("y1", [128, D], F32)
        ysk = T("ysk", [128, D], BF16)
        ss = T("ss", [128, 1], F32)
        vv = T("vv", [128, 1], F32)
        rstd = T("rstd", [128, 1], F32)
        yn = T("yn", [128, D], BF16)
        prev = T("prev", [128, D], F32)
        prevb = T("prevb", [128, D], BF16)
        q = [ps(f"qB{sid}_{i}", [128, 512]) for i in range(4)]
        q2b = q[2].t[:].bitcast(BF16).rearrange("p (c t) -> p c t", c=8)
        smv = lambda r: sm[:, r, :].rearrange("p (j h) -> p j h", j=4)

        for sq in seqs:
            for gi in range(4):
                g = sq * 4 + gi
                t0 = g * 512
                if gi == 0:
                    S.op("pool", lambda e: e.memset(xin[:, :, 0:3], 0.0), w=[xin])
                    S.dma("sp", lambda e: e.dma_start(out=xin[:, :, 3:515],
                          in_=Dd["XBCT"][:, t0:t0 + 512].rearrange("(c p) t -> p c t", p=128)), xin, w=[xin])
                    S.op("pool", lambda e: e.memset(prev[:], 0.0), w=[prev])
                    S.op("pool", lambda e: e.memset(prevb[:], 0.0), w=[prevb])
                else:
                    S.dma("sp", lambda e: e.dma_start(out=xin[:],
                          in_=Dd["XBCT"][:, t0 - 3:t0 + 512].rearrange("(c p) t -> p c t", p=128)), xin, w=[xin])
                S.dma("sp", lambda e: e.dma_start(out=dtr[:], in_=Dd["DTR"][t0:t0 + 512, :].rearrange("(j p) h -> p j h", p=128)),
                      dtr, w=[dtr])
                for c in range(8, 12):
                    pl = q[c - 8]
                    for k in range(4):
                        S.op("pe", _mm(pl[:], diag[:, k * 12 + c, :], xin[:, c, k:k + 512], k == 0, k == 3), r=[diag, xin], w=[pl])
                    dst = BT if c < 10 else CT
                    S.op("act", lambda e, pl=pl, dst=dst, c=c: e.activation(out=dst[:, c % 2, :], in_=pl[:], func=AF.Silu,
                         bias=cbcol[:, c:c + 1]), r=[pl, cbcol], w=[dst])
                yield
                S.op("dve", lambda e: e.tensor_tensor(smv(0), dtr[:], bc(dtb[:].unsqueeze(1), [128, 4, 16]), ALU.add), r=[dtr, dtb], w=[sm])
                S.op("act", lambda e: e.activation(out=sm[:, 1, :], in_=sm[:, 0, :], func=AF.Exp), r=[sm], w=[sm])
                S.op("act", lambda e: e.activation(out=sm[:, 2, :], in_=sm[:, 1, :], func=AF.Ln, bias=1.0), r=[sm], w=[sm])
                S.op("dve", lambda e: e.tensor_tensor(smv(3), smv(2), bc(arow[:].unsqueeze(1), [128, 4, 16]), ALU.mult), r=[sm, arow], w=[sm])
                S.op("pe", _mm(q[0][:, 0:64], trile[:], sm[:, 3, :], True, True), r=[trile, sm], w=[q[0]])
                S.op("pe", _mm(q[0][:, 64:128], onesf[:], sm[:, 3, :], True, True), r=[onesf, sm], w=[q[0]])
                S.op("dve", lambda e: e.tensor_copy(dApad[:, :, 0:16], smv(3)), r=[sm], w=[dApad])
                S.op("dve", lambda e: e.tensor_copy(dApad[:, :, 32:48], smv(3)), r=[sm], w=[dApad])
                for j in range(4):
                    S.op("pe", _mm(q[1][0:48, j * 128:(j + 1) * 128], dApad[:, j, :], trile[:], True, True),
                         r=[trile, dApad], w=[q[1]])
                S.op("dve", lambda e: e.tensor_copy(acs[:], q[0][:, 0:128]), r=[q[0]], w=[acs])
                S.op("dve", lambda e: e.tensor_copy(aHL[0:16, :], q[1][0:16, :]), r=[q[1]], w=[aHL])
                S.op("dve", lambda e: e.tensor_copy(tH[32:48, :], q[1][32:48, :]), r=[q[1]], w=[tH])
                S.op("dve", lambda e: e.tensor_tensor(aHL[32:48, :], q[1][32:48, :], tH[32:48, :], ALU.subtract), r=[q[1], tH], w=[aHL])
                S.op("dve", lambda e: e.tensor_scalar(nHL[:], aHL[:], -1.0, None, ALU.mult), r=[aHL], w=[nHL])
                S.op("act", lambda e: e.activation(out=sm[:, 4, :], in_=acs[:, 0:64], func=AF.Exp), r=[acs], w=[sm])
                S.op("dve", lambda e: e.tensor_tensor(sm[:, 0, :], acs[:, 64:128], acs[:, 0:64], ALU.subtract), r=[acs], w=[sm])
                S.op("act", lambda e: e.activation(out=sm[:, 5, :], in_=sm[:, 0, :], func=AF.Exp), r=[sm], w=[sm])
                S.op("act", lambda e: e.activation(out=sm[:, 6, :], in_=acs[:, 64:128], func=AF.Exp), r=[acs], w=[sm])
                S.op("dve", lambda e: e.tensor_tensor(sm[:, 7, :], sm[:, 2, :], sm[:, 5, :], ALU.mult), r=[sm], w=[sm])
                yield
                for j in range(4):
                    cj = j * 128
                    tt = g * 4 + j
                    hs = slice(j * 16, (j + 1) * 16)
                    S.dma("sp", lambda e: e.dma_start(out=szt[:], in_=Dd["SZ"][tt * 128:(tt + 1) * 128, :]), szt, w=[szt])
                    for hf in range(2):
                        for c4 in range(4):
                            c = 4 * hf + c4
                            o = q[hf][:, c4 * 128:(c4 + 1) * 128]
                            for k in range(4):
                                S.op("pe", _mm(o, xin[:, c, cj + k:cj + k + 128], diag[:, k * 12 + c, :], k == 0, False),
                                     r=[xin, diag], w=[q[hf]])
                            S.op("pe", _mm(o, ones1[0:1, :], cbrow[0:1, c * 128:(c + 1) * 128], False, True), r=[ones1, cbrow], w=[q[hf]])
                        S.op("act", lambda e, hf=hf: e.activation(out=xs_[:, hf * 512:(hf + 1) * 512], in_=q[hf][:], func=AF.Silu),
                             r=[q[hf]], w=[xs_])
                    for c in (8, 9):
                        o = q[2][:, (c - 8) * 128:(c - 7) * 128]
                        for k in range(4):
                            S.op("pe", _mm(o, xin[:, c, cj + k:cj + k + 128], diag[:, k * 12 + c, :], k == 0, False), r=[xin, diag], w=[q[2]])
                        S.op("pe", _mm(o, ones1[0:1, :], cbrow[0:1, c * 128:(c + 1) * 128], False, True), r=[ones1, cbrow], w=[q[2]])
                    S.op("act", lambda e: e.activation(out=bt_[:], in_=q[2][:, 0:256], func=AF.Silu), r=[q[2]], w=[bt_])
                    for g2 in range(2):
                        S.op("pe", _mm(q[2][:, 256 + g2 * 128:384 + g2 * 128], BT[:, g2, cj:cj + 128], CT[:, g2, cj:cj + 128], True, True),
                             r=[BT, CT], w=[q[2]])
                    S.op("dve", lambda e: e.tensor_tensor(gm_[:], q[2][:, 256:512].rearrange("p (g l) -> p g l", g=2),
                         bc(trile[:].unsqueeze(1), [128, 2, 128]), ALU.mult), r=[q[2], trile], w=[gm_])
                    yield
                    xs3 = xs_[:].rearrange("p (h d) -> p h d", h=16)
                    S.op("dve", lambda e: e.tensor_tensor(xdt[:], xs3, bc(sm[:, 2, hs].unsqueeze(2), [128, 16, 64]), ALU.mult),
                         r=[xs_, sm], w=[xdt])
                    S.op("pool", lambda e: e.tensor_tensor(xdtd[:], xs3, bc(sm[:, 7, hs].unsqueeze(2), [128, 16, 64]), ALU.mult),
                         r=[xs_, sm], w=[xdtd])
                    S.op("pool", lambda e: e.tensor_tensor(ysk[:].rearrange("p (h d) -> p h d", h=16), xs3,
                         bc(drow[:].unsqueeze(2), [128, 16, 64]), ALU.mult), r=[xs_, drow], w=[ysk])
                    yield
                    for qq in range(4):
                        sg_ = segq[qq % 2]
                        pl = q[3] if qq % 2 == 0 else q[0]
                        for h4 in range(4):
                            hsel = (4 * qq + h4) * 128
                            S.op("pe", _mm(pl[:, h4 * 128:(h4 + 1) * 128], selb[:, hsel:hsel + 128], aHL[:, cj:cj + 128], h4 == 0, False, True),
                                 r=[selb, aHL], w=[pl])
                        S.op("pe", _mm(pl[:], nHL[:, cj:cj + 128], selb[:, qq * 512:(qq + 1) * 512], False, True, True), r=[selb, nHL], w=[pl])
                        S.op("dve", lambda e, sg_=sg_, pl=pl: e.tensor_scalar(sg_[:], pl[:], 0.0, None, ALU.min), r=[pl], w=[sg_])
                        S.op("act", lambda e, sg_=sg_, qq=qq: e.activation(out=dm[:, 4 * qq:4 * qq + 4, :].rearrange("p h l -> p (h l)"),
                             in_=sg_[:], func=AF.Exp), r=[sg_], w=[dm])
                        yield
                    for g2 in range(2):
                        S.op("dve", lambda e, g2=g2: e.tensor_tensor(Mt[:, 8 * g2:8 * g2 + 8, :], dm[:, 8 * g2:8 * g2 + 8, :],
                             bc(gm_[:, g2:g2 + 1, :], [128, 8, 128]), ALU.mult), r=[dm, gm_], w=[Mt])
                    for hf in range(2):
                        for h8 in range(8):
                            h = 8 * hf + h8
                            S.op("pe", _mm(q[2 + hf][:, h8 * 64:(h8 + 1) * 64], Mt[:, h, :], xdt[:, h, :], h8 == 0, False, True),
                                 r=[Mt, xdt], w=[q[2 + hf]])
                        S.op("pe", _mm(q[2 + hf][:], identb[:], ysk[:, hf * 512:(hf + 1) * 512], False, True, True), r=[identb, ysk], w=[q[2 + hf]])
                    for hf in range(2):
                        S.op("pe", _mm(q[hf][:], CT[:, hf, cj:cj + 128], prevb[:, hf * 512:(hf + 1) * 512], True, True),
                             r=[CT, prevb], w=[q[hf]])
                    yield
                    for hf in range(2):
                        sl = slice(hf * 512, (hf + 1) * 512)
                        S.op("dve", lambda e, hf=hf, sl=sl: e.tensor_tensor(y1[:, sl].rearrange("p (h d) -> p h d", h=8),
                             q[hf][:].rearrange("p (h d) -> p h d", h=8),
                             bc(sm[:, 4, j * 16 + 8 * hf:j * 16 + 8 * hf + 8].unsqueeze(2), [128, 8, 64]), ALU.mult),
                             r=[q[hf], sm], w=[y1])
                        S.op("dve", lambda e, hf=hf, sl=sl: e.tensor_tensor(y1[:, sl], q[2 + hf][:], y1[:, sl], ALU.add),
                             r=[q[2 + hf], y1], w=[y1])
                    S.op("dve", lambda e: e.tensor_tensor(y1[:], y1[:], szt[:], ALU.mult), r=[y1, szt], w=[y1])
                    yield
                    for hf in range(2):
                        S.op("pe", _mm(q[hf][:], bt_[:, hf * 128:(hf + 1) * 128],
                             xdtd[:, 8 * hf:8 * hf + 8, :].rearrange("p h d -> p (h d)"), True, True), r=[bt_, xdtd], w=[q[hf]])
                    S.op("pool", lambda e: e.tensor_tensor(pvb[:].rearrange("p (h d) -> p h d", h=16),
                         prev[:].rearrange("p (h d) -> p h d", h=16), bc(sm[:, 6, hs].unsqueeze(2), [128, 16, 64]), ALU.mult),
                         r=[prev, sm], w=[pvb])
                    for hf in range(2):
                        sl = slice(hf * 512, (hf + 1) * 512)
                        S.op("dve", lambda e, hf=hf, sl=sl: e.tensor_tensor(prev[:, sl], pvb[:, sl], q[hf][:], ALU.add),
                             r=[pvb, q[hf]], w=[prev])
                    S.op("act", lambda e: e.copy(prevb[:], prev[:]), r=[prev], w=[prevb])
                    yield
                    S.op("act", lambda e: e.activation(out=junk[:], in_=y1[:], func=AF.Square, accum_out=ss[:, 0:1]), r=[y1], w=[junk, ss])
                    rstd_from_ss(K, ss, rstd, vv, neghalf)
                    S.op("act", lambda e: e.activation(out=yn[:], in_=y1[:], func=AF.Copy, scale=rstd[:, 0:1]), r=[y1, rstd], w=[yn])
                    for c in range(8):
                        S.op("pe", lambda e, c=c: e.transpose(q2b[:, c, :], yn[:, c * 128:(c + 1) * 128], identb[:]), r=[yn, identb], w=[q[2]])
                    S.op("act", lambda e: e.copy(ynT[:, :, cj:cj + 128], q2b), r=[q[2]], w=[ynT])
                    yield
                for n in range(8):
                    po = q[n % 4]
                    gt_ = gtn[n % 2]
                    S.dma("sp", lambda e, gt_=gt_, n=n: e.dma_start(out=gt_[:], in_=Dd["GATET"][n * 128:(n + 1) * 128, t0:t0 + 512]),
                          gt_, w=[gt_])
                    for kc in range(8):
                        S.op("pe", _mm(po[:], wssd[:, kc, n * 128:(n + 1) * 128], ynT[:, kc, :], kc == 0, kc == 7), r=[wssd, ynT], w=[po])
                    S.op("dve", lambda e, po=po, n=n, gt_=gt_: e.tensor_tensor(stg[:, n % 4, :], po[:], gt_[:], ALU.mult),
                         r=[po, gt_], w=[stg])
                    if n % 4 == 3:
                        r0 = (n - 3) * 128
                        S.dma("sp", lambda e, r0=r0: e.dma_start(
                            out=Dd["GS1T"][r0:r0 + 512, t0:t0 + 512].rearrange("(c p) t -> p c t", p=128), in_=stg[:]), stg, r=[stg])
                    yield

    ns = K.nseq
    if ns >= 2:
        half = ns // 2
        gens = [stream(0, list(range(0, half))), stream(1, list(range(half, ns)))]
        for _ in range(B_OFFSET):
            next(gens[0])
    else:
        gens = [stream(0, [0])]
    run_streams(gens)


def phase_CDE(K, pes, upto):
    nc, S, I, Dd = K.nc, K.S, K.I, K.Dd
    NT, NBLK = K.NT, K.NBLK
    sbP, _ = mk_alloc(K, pes)
    CP = load_consts(K, sbP, ["neghalf", "iota", "bpos", "ident_bf"])
    neghalf, identb = CP["neghalf"], CP["ident_bf"]
    lgall = sbP("lgall", [128, NT, 36], F32)
    d1i = sbP("d1i", [128, 2, NT], I32)
    idxw = sbP("idxw", [128, 6, NBLK], I32)
    gw = sbP("gw", [128, 2, NT], F32)
    with contextlib.ExitStack() as pc:
        phase_C(K, pc, dict(lgall=lgall, neghalf=neghalf, identb=identb))
        S.barrier()
    if upto < "D":
        return
    with contextlib.ExitStack() as pr_:
        sbR, _ = mk_alloc(K, pr_)
        oh1 = sbR("oh1", [128, NT, 32], BF16)
        oh2 = sbR("oh2", [128, NT, 32], BF16)
        rk = sbR("rk", [128, 2, NT], F32)
        run = sbR("run", [128, 32], F32)
        with contextlib.ExitStack() as pc2:
            phase_C2(K, pc2, dict(lgall=lgall, oh1=oh1, oh2=oh2, rk=rk, gw=gw, run=run))
            S.barrier()
        with contextlib.ExitStack() as pd:
            phase_D(K, pd, dict(oh1=oh1, oh2=oh2, rk=rk, gw=gw, run=run, d1i=d1i, idxw=idxw, iota=CP["iota"], bpos=CP["bpos"],
                                identb=identb))
            S.barrier()
    if upto < "E":
        return
    with contextlib.ExitStack() as pe_:
        phase_E(K, pe_, dict(gw=gw, d1i=d1i, neghalf=neghalf, identb=identb))


def phase_C(K, pes, P):
    nc, S, I, Dd = K.nc, K.S, K.I, K.Dd
    identb, neghalf, lgall = P["identb"], P["neghalf"], P["lgall"]
    with contextlib.ExitStack() as es1:
        sb, ps = mk_alloc(K, es1)
        C = load_consts(K, sb, ["ident_f", "onesdiv_bf"])
        identf, onesdiv = C["ident_f"], C["onesdiv_bf"]
        cw = sb("cw31", [128, 31, 8], F32, dma=True)
        with nc.allow_non_contiguous_dma(reason="small conv weights"):
            S.dma("sp", lambda e: e.dma_start(out=cw[:], in_=I["conf_dw_w"].rearrange("k (c p) -> p k c", p=128)), cw, w=[cw])
        diag = sb("diag31", [128, 248, 128], BF16)
        for k in range(31):
            S.op("dve", lambda e, k=k: e.tensor_tensor(diag[:, k * 8:(k + 1) * 8, :],
                 bc(identf[:].unsqueeze(1), [128, 8, 128]), bc(cw[:, k, :].unsqueeze(2), [128, 8, 128]), ALU.mult), r=[identf, cw], w=[diag])
        cb31 = load_colvec(K, sb, "cb31", I["conf_dw_b"], 8)
        lng = load_colvec(K, sb, "lng", I["conf_ln_g"], 8)
        lnb = load_colvec(K, sb, "lnb", I["conf_ln_b"], 8)
        wconf = load_weight_bf(K, sb, "wconf", I["w_conf_out"], D, D)

        def stream1(sid, groups):
            def T(n, shape, dt, dma=False):
                return sb(f"{n}C{sid}", shape, dt, dma=dma)
            gl = T("glu", [128, 8, 542], BF16, True)
            gcn = [T(f"gcn{i}", [128, 512], BF16, True) for i in range(2)]
            g1n = [T(f"g1n{i}", [128, 512], BF16, True) for i in range(2)]
            cT = T("cT", [128, 8, 512], BF16)
            csq = [T(f"csq{i}", [128, 512], BF16) for i in range(2)]
            mean = T("mean", [128, 512], F32)
            rstdb = T("rstdb", [128, 512], F32)
            cn = [T(f"cn{i}", [128, 512], F32) for i in range(2)]
            var = cn[0]
            csT = T("csT", [128, 8, 512], BF16)
            stg = T("stg", [128, 4, 512], BF16, True)
            pc_ = ps(f"pcC{sid}", [128, 512])
            pm = [ps(f"pmC{sid}_{i}", [128, 512]) for i in range(2)]
            po = ps(f"poC{sid}", [128, 512])
            for g in groups:
                gi = g % 4
                t0 = g * 512
                if gi == 0:
                    S.op("pool", lambda e: e.memset(gl[:, :, 0:30], 0.0), w=[gl])
                    S.dma("sp", lambda e: e.dma_start(out=gl[:, :, 30:542],
                          in_=Dd["GLUT"][:, t0:t0 + 512].rearrange("(c p) t -> p c t", p=128)), gl, w=[gl])
                else:
                    S.dma("sp", lambda e: e.dma_start(out=gl[:],
                          in_=Dd["GLUT"][:, t0 - 30:t0 + 512].rearrange("(c p) t -> p c t", p=128)), gl, w=[gl])
                for c in range(8):
                    for k in range(31):
                        S.op("pe", _mm(pc_[:], diag[:, k * 8 + c, :], gl[:, c, k:k + 512], k == 0, k == 30), r=[diag, gl], w=[pc_])
                    S.op("act", lambda e, c=c: e.activation(out=cT[:, c, :], in_=pc_[:], func=AF.Identity, bias=cb31[:, c:c + 1]),
                         r=[pc_, cb31], w=[cT])
                    cq = csq[c % 2]
                    S.op("dve", lambda e, c=c, cq=cq: e.tensor_tensor(cq[:], cT[:, c, :], cT[:, c, :], ALU.mult), r=[cT], w=[cq])
                    S.op("pe", _mm(pm[0][:], onesdiv[:], cT[:, c, :], c == 0, c == 7), r=[onesdiv, cT], w=[pm[0]])
                    S.op("pe", _mm(pm[1][:], onesdiv[:], cq[:], c == 0, c == 7), r=[onesdiv, cq], w=[pm[1]])
                    yield
                S.op("dve", lambda e: e.tensor_copy(mean[:], pm[0][:]), r=[pm[0]], w=[mean])
                S.op("dve", lambda e: e.tensor_tensor(var[:], mean[:], mean[:], ALU.mult), r=[mean], w=[var])
                S.op("dve", lambda e: e.tensor_tensor(var[:], pm[1][:], var[:], ALU.subtract), r=[pm[1], var], w=[var])
                S.op("dve", lambda e: e.tensor_scalar(var[:], var[:], 0.0, EPS, ALU.max, ALU.add), r=[var], w=[var])
                S.op("act", lambda e: e.activation(out=rstdb[:], in_=var[:], func=AF.Ln), r=[var], w=[rstdb])
                S.op("act", lambda e: e.activation(out=rstdb[:], in_=rstdb[:], func=AF.Exp, scale=-0.5), r=[rstdb], w=[rstdb])
                yield
                for c in range(8):
                    cn_ = cn[c % 2]
                    S.op("dve", lambda e, c=c, cn_=cn_: e.tensor_tensor(cn_[:], cT[:, c, :], mean[:], ALU.subtract), r=[cT, mean], w=[cn_])
                    S.op("dve", lambda e, cn_=cn_: e.tensor_tensor(cn_[:], cn_[:], rstdb[:], ALU.mult), r=[cn_, rstdb], w=[cn_])
                    S.op("act", lambda e, c=c, cn_=cn_: e.activation(out=csT[:, c, :], in_=cn_[:], func=AF.Silu, bias=lnb[:, c:c + 1],
                         scale=lng[:, c:c + 1]), r=[cn_, lnb, lng], w=[csT])
                    if c % 2 == 1:
                        yield
                for n in range(8):
                    gc_, g1_ = gcn[n % 2], g1n[n % 2]
                    S.dma("sp", lambda e, gc_=gc_, n=n: e.dma_start(out=gc_[:], in_=Dd["GATET"][D + n * 128:D + (n + 1) * 128, t0:t0 + 512]),
                          gc_, w=[gc_])
                    S.dma("sp", lambda e, g1_=g1_, n=n: e.dma_start(out=g1_[:], in_=Dd["GS1T"][n * 128:(n + 1) * 128, t0:t0 + 512]),
                          g1_, w=[g1_])
                    for kc in range(8):
                        S.op("pe", _mm(po[:], wconf[:, kc, n * 128:(n + 1) * 128], csT[:, kc, :], kc == 0, kc == 7), r=[wconf, csT], w=[po])
                    tf = cn[n % 2]
                    S.op("dve", lambda e, tf=tf, gc_=gc_: e.tensor_tensor(tf[:], po[:], gc_[:], ALU.mult), r=[po, gc_], w=[tf])
                    S.op("dve", lambda e, tf=tf, n=n, g1_=g1_: e.tensor_tensor(stg[:, n % 4, :], tf[:], g1_[:], ALU.add),
                         r=[tf, g1_], w=[stg])
                    if n % 4 == 3:
                        r0 = (n - 3) * 128
                        S.dma("sp", lambda e, r0=r0: e.dma_start(
                            out=Dd["GST"][r0:r0 + 512, t0:t0 + 512].rearrange("(c p) t -> p c t", p=128), in_=stg[:]), stg, r=[stg])
                    yield

        gens = [stream1(0, list(range(0, K.NG, 2))), stream1(1, list(range(1, K.NG, 2)))]
        for _ in range(C1_OFFSET):
            next(gens[0])
        run_streams(gens)
        S.barrier()

    with contextlib.ExitStack() as es2:
        sb, ps = mk_alloc(K, es2)
        wo = load_weight_bf(K, sb, "wo", I["w_o"], D, D)
        gffn = load_rowbc(K, sb, "gffn", I["norm_ffn_g"], D)
        wrh = sb("wrh", [128, 8, 36], BF16)
        wrl = sb("wrl", [128, 8, 36], BF16)
        wr0 = sb("wr0", [128, 8, 36], F32, dma=True)
        S.dma("sp", lambda e: e.dma_start(out=wr0[:], in_=I["router_w"].rearrange("(p j) e -> p j e", j=8)), wr0, w=[wr0])
        S.op("dve", lambda e: e.tensor_copy(wrh[:], wr0[:]), r=[wr0], w=[wrh])
        S.op("dve", lambda e: e.tensor_tensor(wrl[:], wr0[:], wrh[:], ALU.subtract), r=[wr0, wrh], w=[wrl])
        rb = load_rowbc(K, sb, "rb", I["router_b"], 36)
        junk = sb("junkC", [128, D], BF16)

        def stream2(sid, tiles):
            def T(n, shape, dt, dma=False):
                return sb(f"{n}D{sid}", shape, dt, dma=dma)
            xL = [T(f"xt{i}", [128, D], F32, True) for i in range(2)]
            gL = [T(f"gst{i}", [128, 8, 128], BF16, True) for i in range(2)]
            h_ = T("h1t", [128, D], F32, True)
            u2_ = T("u2f", [128, D], F32)
            hi_ = T("hi", [128, D], BF16, True)
            lo_ = T("lo", [128, D], BF16)
            hT_ = T("hiT", [128, 8, 128], BF16)
            lT_ = T("loT", [128, 8, 128], BF16)
            ss_ = T("ss", [128, 1], F32)
            vv_ = T("vv", [128, 1], F32)
            rs_ = T("rs", [128, 1], F32)
            pA_ = ps(f"poD{sid}", [128, 512])
            pr_ = ps(f"prD{sid}", [128, 512])
            po = [pA_, pA_]
            pl = pA_
            prb_ = pr_.t[:].bitcast(BF16).rearrange("p (c t) -> p c t", c=8)
            def loads(i):
                t = tiles[i]
                x_, gst = xL[i % 2], gL[i % 2]
                S.dma("sp", lambda e: e.dma_start(out=x_[:], in_=I["x"][t * 128:(t + 1) * 128, :]), x_, w=[x_])
                S.dma("sp", lambda e: e.dma_start(out=gst[:], in_=Dd["GST"][:, t * 128:(t + 1) * 128].rearrange("(c p) t -> p c t", p=128)),
                      gst, w=[gst])

            loads(0)
            for i, t in enumerate(tiles):
                x_, gst = xL[i % 2], gL[i % 2]
                if i + 1 < len(tiles):
                    loads(i + 1)
                for h in range(2):
                    for kc in range(8):
                        S.op("pe", _mm(po[h][:], gst[:, kc, :], wo[:, kc, h * 512:(h + 1) * 512], kc == 0, kc == 7), r=[gst, wo], w=[po[h]])
                    S.op("dve", lambda e, h=h: e.tensor_tensor(h_[:, h * 512:(h + 1) * 512], po[h][:], x_[:, h * 512:(h + 1) * 512], ALU.add),
                         r=[po[h], x_], w=[h_])
                S.dma("sp", lambda e: e.dma_start(out=Dd["H1"][t * 128:(t + 1) * 128, :], in_=h_[:]), h_, r=[h_])
                yield
                S.op("act", lambda e: e.activation(out=junk[:], in_=h_[:], func=AF.Square, accum_out=ss_[:, 0:1]), r=[h_], w=[junk, ss_])
                rstd_from_ss(K, ss_, rs_, vv_, neghalf)
                S.op("dve", lambda e: e.scalar_tensor_tensor(u2_[:], h_[:], rs_[:, 0:1], gffn[:], ALU.mult, ALU.mult),
                     r=[h_, rs_, gffn], w=[u2_])
                yield
                u2p = u2_[:].rearrange("t (p j) -> t j p", j=8)
                S.op("act", lambda e: e.copy(hi_[:].rearrange("t (j p) -> t j p", j=8), u2p), r=[u2_], w=[hi_])
                S.op("dve", lambda e: e.tensor_tensor(lo_[:].rearrange("t (j p) -> t j p", j=8), u2p,
                     hi_[:].rearrange("t (j p) -> t j p", j=8), ALU.subtract), r=[u2_, hi_], w=[lo_])
                S.dma("sp", lambda e: e.dma_start(out=Dd["U2"][t * 128:(t + 1) * 128, :], in_=hi_[:]), hi_, r=[hi_])
                yield
                for src, dstT in ((hi_, hT_), (lo_, lT_)):
                    for c in range(8):
                        S.op("pe", lambda e, c=c, src=src: e.transpose(prb_[:, c, :], src[:, c * 128:(c + 1) * 128], identb[:]),
                             r=[src, identb], w=[pr_])
                    S.op("act", lambda e, dstT=dstT: e.copy(dstT[:], prb_), r=[pr_], w=[dstT])
                    yield
                n_mm = 0
                for (aT, wb_) in ((hT_, wrh), (lT_, wrh), (hT_, wrl)):
                    for c in range(8):
                        S.op("pe", _mm(pl[:, 0:36], aT[:, c, :], wb_[:, c, :], n_mm == 0, n_mm == 23), r=[aT, wb_], w=[pl])
                        n_mm += 1
                S.op("dve", lambda e: e.tensor_tensor(lgall[:, t, :], pl[:, 0:36], rb[:], ALU.add), r=[pl, rb], w=[lgall])
                yield

        gens = [stream2(i, list(range(i, K.NT, 4))) for i in range(4)]
        for i in range(3):
            for _ in range(2 * (3 - i)):
                next(gens[i])
        run_streams(gens)
        S.barrier()


def phase_C2(K, pes, P):
    nc, S, I, Dd = K.nc, K.S, K.I, K.Dd
    NT = K.NT
    sb, ps = mk_alloc(K, pes)
    C = load_consts(K, sb, ["ones_bf", "ustr_bf"])
    onesb, ustr = C["ones_bf"], C["ustr_bf"]
    lg, oh1, oh2, rk, gw, run = (P[k] for k in ("lgall", "oh1", "oh2", "rk", "gw", "run"))
    sc = sb("scR", [128, 10, NT], F32)
    gmask = sb("gmaskR", [128, NT, 4], F32)
    g4 = sb("g4R", [128, NT, 4], F32)
    ein = sb("einR", [128, NT, 8], F32)
    ein2 = sb("ein2R", [128, NT, 8], F32)
    t8 = sb("t8R", [128, NT, 8], F32)
    m1k = sb("m1kR", [128, NT, 8], F32)
    m2k = sb("m2kR", [128, NT, 8], F32)
    ohs = sb("ohsR", [128, NT, 32], BF16)
    Pf = sb("PfR", [128, NT, 32], F32)
    cntf = sb("cntfR", [128, NT, 32], F32)
    base = sb("baseR", [128, NT, 32], F32)
    tmp = sb("tmpR", [128, NT, 32], F32)
    nq = (NT * 32 + 511) // 512
    pp = [ps(f"ppR{i}", [128, 512]) for i in range(min(nq, 4))]
    pcn = [ps(f"pcR{i}", [128, 512]) for i in range(min(nq, 4))]
    lgG = lg[:, :, 0:4]
    key8 = sb("key8R", [128, NT, 8], F32)
    for e_ in range(8):
        S.op("pool", lambda e, e_=e_: e.memset(key8[:, :, e_:e_ + 1], float(8 - e_)), w=[key8])
    kt = sb("ktR", [128, NT, 8], F32)
    kmx = sb("kmxR", [128, NT], F32)

    def first_only(mask, n):
        S.op("dve", lambda e: e.tensor_tensor(kt[:, :, 0:n], mask[:], key8[:, :, 0:n], ALU.mult), r=[mask, key8], w=[kt])
        S.op("dve", lambda e: e.reduce_max(kmx[:], kt[:, :, 0:n], AX.X), r=[kt], w=[kmx])
        S.op("dve", lambda e: e.tensor_tensor(mask[:], kt[:, :, 0:n], bc(kmx[:].unsqueeze(2), [128, NT, n]), ALU.is_equal),
             r=[kt, kmx], w=[mask])

    S.op("dve", lambda e: e.reduce_max(sc[:, 0, :], lgG, AX.X), r=[lg], w=[sc])
    S.op("dve", lambda e: e.tensor_tensor(gmask[:], lgG, bc(sc[:, 0, :].unsqueeze(2), [128, NT, 4]), ALU.is_equal), r=[lg, sc], w=[gmask])
    first_only(gmask, 4)
    S.op("dve", lambda e: e.tensor_tensor(g4[:], lgG, bc(sc[:, 0, :].unsqueeze(2), [128, NT, 4]), ALU.subtract), r=[lg, sc], w=[g4])
    S.op("act", lambda e: e.activation(out=g4[:], in_=g4[:], func=AF.Exp), r=[g4], w=[g4])
    S.op("dve", lambda e: e.reduce_sum(sc[:, 1, :], g4[:], AX.X), r=[g4], w=[sc])
    S.op("dve", lambda e: e.reciprocal(sc[:, 2, :], sc[:, 1, :]), r=[sc], w=[sc])
    lgE = lg[:, :, 4:36].rearrange("p t (g e) -> p t g e", g=4)
    for g in range(4):
        dst = ein if g == 0 else t8
        S.op("dve", lambda e, g=g, dst=dst: e.tensor_tensor(dst[:], lgE[:, :, g, :], bc(gmask[:, :, g:g + 1], [128, NT, 8]), ALU.mult),
             r=[lg, gmask], w=[dst])
        if g > 0:
            S.op("dve", lambda e: e.tensor_tensor(ein[:], ein[:], t8[:], ALU.add), r=[ein, t8], w=[ein])
    S.op("dve", lambda e: e.reduce_max(sc[:, 3, :], ein[:], AX.X), r=[ein], w=[sc])
    S.op("dve", lambda e: e.tensor_tensor(m1k[:], ein[:], bc(sc[:, 3, :].unsqueeze(2), [128, NT, 8]), ALU.is_equal), r=[ein, sc], w=[m1k])
    first_only(m1k, 8)
    S.op("dve", lambda e: e.scalar_tensor_tensor(ein2[:], m1k[:], -1e30, ein[:], ALU.mult, ALU.add), r=[m1k, ein], w=[ein2])
    S.op("dve", lambda e: e.reduce_max(sc[:, 4, :], ein2[:], AX.X), r=[ein2], w=[sc])
    S.op("dve", lambda e: e.tensor_tensor(m2k[:], ein2[:], bc(sc[:, 4, :].unsqueeze(2), [128, NT, 8]), ALU.is_equal), r=[ein2, sc], w=[m2k])
    first_only(m2k, 8)
    S.op("dve", lambda e: e.tensor_tensor(sc[:, 5, :], sc[:, 4, :], sc[:, 3, :], ALU.subtract), r=[sc], w=[sc])
    S.op("act", lambda e: e.activation(out=sc[:, 6, :], in_=sc[:, 5, :], func=AF.Exp), r=[sc], w=[sc])
    S.op("dve", lambda e: e.tensor_scalar(sc[:, 7, :], sc[:, 6, :], 1.0, None, ALU.add), r=[sc], w=[sc])
    S.op("dve", lambda e: e.reciprocal(sc[:, 8, :], sc[:, 7, :]), r=[sc], w=[sc])
    S.op("dve", lambda e: e.tensor_tensor(gw[:, 0, :], sc[:, 8, :], sc[:, 2, :], ALU.mult), r=[sc], w=[gw])
    S.op("dve", lambda e: e.tensor_tensor(gw[:, 1, :], sc[:, 2, :], gw[:, 0, :], ALU.subtract), r=[sc, gw], w=[gw])
    for (ohX, mk) in ((oh1, m1k), (oh2, m2k)):
        ohv = ohX[:].rearrange("p t (g e) -> p t g e", g=4)
        for g in range(4):
            S.op("dve", lambda e, g=g, ohv=ohv, mk=mk: e.tensor_tensor(ohv[:, :, g, :], mk[:], bc(gmask[:, :, g:g + 1], [128, NT, 8]), ALU.mult),
                 r=[gmask, mk], w=[ohX])
    S.op("dve", lambda e: e.tensor_tensor(ohs[:], oh1[:], oh2[:], ALU.add), r=[oh1, oh2], w=[ohs])
    ohsf = ohs[:].rearrange("p t e -> p (t e)")
    Pff = Pf[:].rearrange("p t e -> p (t e)")
    cnf = cntf[:].rearrange("p t e -> p (t e)")
    ncol = NT * 32
    for q in range(nq):
        c0, c1 = q * 512, min(ncol, (q + 1) * 512)
        a, b_ = pp[q % len(pp)], pcn[q % len(pcn)]
        S.op("pe", _mm(a[:, 0:c1 - c0], ustr[:], ohsf[:, c0:c1], True, True), r=[ustr, ohs], w=[a])
        S.op("pe", _mm(b_[:, 0:c1 - c0], onesb[:], ohsf[:, c0:c1], True, True), r=[onesb, ohs], w=[b_])
        S.op("dve", lambda e, a=a, c0=c0, c1=c1: e.tensor_copy(Pff[:, c0:c1], a[:, 0:c1 - c0]), r=[a], w=[Pf])
        S.op("act", lambda e, b_=b_, c0=c0, c1=c1: e.copy(cnf[:, c0:c1], b_[:, 0:c1 - c0]), r=[b_], w=[cntf])
    cur, oth = cntf, base
    src0 = cntf
    d_ = 1
    bufs = [base, tmp]
    bi = 0
    cur = cntf
    while d_ < NT:
        nxt = bufs[bi % 2]
        bi += 1
        S.op("dve", lambda e, cur=cur, nxt=nxt, d_=d_: e.tensor_copy(nxt[:, 0:d_, :], cur[:, 0:d_, :]), r=[cur], w=[nxt])
        S.op("dve", lambda e, cur=cur, nxt=nxt, d_=d_: e.tensor_tensor(nxt[:, d_:NT, :], cur[:, d_:NT, :], cur[:, 0:NT - d_, :], ALU.add),
             r=[cur], w=[nxt])
        cur = nxt
        d_ *= 2
    S.op("dve", lambda e, cur=cur: e.tensor_copy(run[:], cur[:, NT - 1, :]), r=[cur], w=[run])
    if cur is base:
        S.op("dve", lambda e: e.tensor_tensor(base[:], base[:], cntf[:], ALU.subtract), r=[base, cntf], w=[base])
    else:
        S.op("dve", lambda e, cur=cur: e.tensor_tensor(base[:], cur[:], cntf[:], ALU.subtract), r=[cur, cntf], w=[base])
    S.op("dve", lambda e: e.tensor_tensor(Pf[:], Pf[:], base[:], ALU.add), r=[Pf, base], w=[Pf])
    for kk, ohX in ((0, oh1), (1, oh2)):
        S.op("dve", lambda e, ohX=ohX: e.tensor_tensor(tmp[:], Pf[:], ohX[:], ALU.mult), r=[Pf, ohX], w=[tmp])
        S.op("dve", lambda e, kk=kk: e.reduce_sum(rk[:, kk, :], tmp[:], AX.X), r=[tmp], w=[rk])


def phase_D(K, pes, P):
    nc, S, I, Dd = K.nc, K.S, K.I, K.Dd
    NT, NBLK = K.NT, K.NBLK
    sb, ps = mk_alloc(K, pes)
    oh1, oh2, rk, run, d1i, idxw, iota, bpos, identb = (P[k] for k in ("oh1", "oh2", "rk", "run", "d1i", "idxw", "iota", "bpos", "identb"))
    ci = sb("ciD", [128, 32], I32)
    padded = sb("paddedD", [128, 32], F32)
    pend = sb("pendD", [128, 32], F32)
    pstart = sb("pstartD", [128, 32], F32)
    big = sb("bigD", [128, max(NT, NBLK), 32], F32)
    df = sb("dfD", [128, 2, NT], F32)
    be = sb("beD", [128, NBLK], F32)
    bf_ = sb("bfD", [128, 6, NBLK], F32)
    S.op("dve", lambda e: e.tensor_scalar(padded[:], run[:], float(MB - 1), None, ALU.add), r=[run], w=[padded])
    S.op("dve", lambda e: e.tensor_copy(ci[:], padded[:]), r=[padded], w=[ci])
    SH = MB.bit_length() - 1
    S.op("dve", lambda e: e.tensor_scalar(ci[:], ci[:], SH, None, ALU.arith_shift_right), r=[ci], w=[ci])
    S.op("dve", lambda e: e.tensor_scalar(ci[:], ci[:], SH, None, ALU.logical_shift_left), r=[ci], w=[ci])
    S.op("dve", lambda e: e.tensor_copy(padded[:], ci[:]), r=[ci], w=[padded])
    S.op("dve", lambda e: e.tensor_copy(pend[:, 0:1], padded[:, 0:1]), r=[padded], w=[pend])
    for e_ in range(1, 32):
        S.op("dve", lambda e, e_=e_: e.tensor_tensor(pend[:, e_:e_ + 1], pend[:, e_ - 1:e_], padded[:, e_:e_ + 1], ALU.add),
             r=[pend, padded], w=[pend])
    S.op("dve", lambda e: e.tensor_tensor(pstart[:], pend[:], padded[:], ALU.subtract), r=[pend, padded], w=[pstart])
    for kk, ohX in ((0, oh1), (1, oh2)):
        S.op("dve", lambda e, ohX=ohX: e.tensor_tensor(big[:, 0:NT, :], ohX[:], bc(pstart[:].unsqueeze(1), [128, NT, 32]), ALU.mult),
             r=[ohX, pstart], w=[big])
        S.op("dve", lambda e, kk=kk: e.reduce_sum(df[:, kk, :], big[:, 0:NT, :], AX.X), r=[big], w=[df])
    S.op("dve", lambda e: e.tensor_tensor(df[:], df[:], rk[:], ALU.add), r=[df, rk], w=[df])
    S.op("dve", lambda e: e.tensor_copy(d1i[:], df[:]), r=[df], w=[d1i])
    S.op("dve", lambda e: e.tensor_tensor(big[:, 0:NBLK, :], bc(pend[:].unsqueeze(1), [128, NBLK, 32]),
         bc(bpos[:].unsqueeze(2), [128, NBLK, 32]), ALU.is_le), r=[pend, bpos], w=[big])
    S.op("dve", lambda e: e.reduce_sum(be[:], big[:, 0:NBLK, :], AX.X), r=[big], w=[be])
    S.op("dve", lambda e: e.tensor_scalar(be[:], be[:], 31.0, None, ALU.min), r=[be], w=[be])
    for q in range(6):
        if q < 2:
            S.op("dve", lambda e, q=q: e.tensor_scalar(bf_[:, q, :], be[:], 256.0, float(q), ALU.mult, ALU.add), r=[be], w=[bf_])
            S.op("dve", lambda e, q=q: e.scalar_tensor_tensor(bf_[:, q, :], bc(iota[:, 0:1], [128, NBLK]), 2.0, bf_[:, q, :], ALU.mult, ALU.add),
                 r=[iota, bf_], w=[bf_])
        else:
            S.op("dve", lambda e, q=q: e.tensor_scalar(bf_[:, q, :], be[:], 512.0, float((q - 2) * 128), ALU.mult, ALU.add), r=[be], w=[bf_])
            S.op("dve", lambda e, q=q: e.tensor_tensor(bf_[:, q, :], bf_[:, q, :], bc(iota[:, 0:1], [128, NBLK]), ALU.add),
                 r=[iota, bf_], w=[bf_])
    chg = sb("chgD", [128, NBLK], F32)
    HB = NBLK // 2
    S.op("dve", lambda e: e.memset(chg[:], 1.0), w=[chg])
    S.op("dve", lambda e: e.tensor_tensor(chg[:, 1:HB], be[:, 1:HB], be[:, 0:HB - 1], ALU.not_equal), r=[be], w=[chg])
    S.op("dve", lambda e: e.tensor_tensor(chg[:, HB + 1:NBLK], be[:, HB + 1:NBLK], be[:, HB:NBLK - 1], ALU.not_equal), r=[be], w=[chg])
    S.op("dve", lambda e: e.tensor_scalar(chg[:], chg[:], -float(2 ** 30), float(2 ** 30), ALU.mult, ALU.add), r=[chg], w=[chg])
    S.op("dve", lambda e: e.tensor_tensor(bf_[:], bf_[:], bc(chg[:].unsqueeze(1), [128, 6, NBLK]), ALU.add), r=[bf_, chg], w=[bf_])
    S.op("dve", lambda e: e.tensor_copy(idxw[:], bf_[:]), r=[bf_], w=[idxw])
    with contextlib.ExitStack() as ses:
        sbs, _ = mk_alloc(K, ses)
        TG = 8
        u2g = [sbs(f"u2gD{i}", [128, TG, D], BF16, dma=True) for i in range(NT // TG)]
        for gi_, u_ in enumerate(u2g):
            S.dma("sp", lambda e, u_=u_, gi_=gi_: e.dma_start(out=u_[:],
                  in_=Dd["U2"][gi_ * TG * 128:(gi_ + 1) * TG * 128, :].rearrange("(j p) d -> p j d", p=128)), u_, w=[u_])
        scat = S.buf("scatD", None, dma="sw")
        for t in range(NT):
            u_ = u2g[t // TG]
            for kk in range(2):
                S.dma("pool", lambda e, u_=u_, t=t, kk=kk: e.indirect_dma_start(out=Dd["XS"],
                      out_offset=bass.IndirectOffsetOnAxis(ap=d1i[:, kk, t:t + 1], axis=0), in_=u_[:, t % TG, :], in_offset=None),
                      scat, r=[u_, d1i])
        S.barrier()
    breg = nc.gpsimd.to_reg(NEXP * DEXP - 1)

    def streamD(sid, blocks):
        def T(n, shape, dt, dma=False):
            return sb(f"{n}X{sid}", shape, dt, dma=dma)
        wg_ = T("wg", [128, 8, 512], BF16, "sw")
        wu_ = T("wu", [128, 8, 512], BF16, "sw")
        wd_ = T("wd", [128, 4, D], BF16, "sw")
        NS = MB // 128
        xb_ = T("xb", [128, NS, D], BF16, True)
        xT = T("xT", [128, 8, MB], BF16)
        sg = [T(f"sg{i}", [128, MB], F32) for i in range(2)]
        hT = T("hT", [128, 4, MB], BF16)
        yt = [T(f"yt{i}", [128, D], BF16, True) for i in range(2)]
        bk = [ps(f"bkX{sid}_{i}", [128, 512]) for i in range(4)]
        pr = bk[0]
        prb = pr.t[:].bitcast(BF16).rearrange("p (c t) -> p c t", c=8)
        nyt = 0
        for b in blocks:
            for (wt_, src) in ((wg_, I["expert_w_gate"]), (wu_, I["expert_w_up"])):
                for q in range(2):
                    S.dma("pool", lambda e, wt_=wt_, src=src, q=q: e.indirect_dma_start(
                        out=wt_[:, 4 * q:4 * q + 4, :].rearrange("p j f -> p (j f)"), out_offset=None, in_=src,
                        in_offset=bass.IndirectOffsetOnAxis(ap=idxw[:, q, b:b + 1], axis=0),
                        bounds_check=breg, oob_is_err=False), wt_, r=[idxw], w=[wt_])
            for fc in range(4):
                S.dma("pool", lambda e, fc=fc: e.indirect_dma_start(out=wd_[:, fc, :], out_offset=None, in_=I["expert_w_down"],
                      in_offset=bass.IndirectOffsetOnAxis(ap=idxw[:, 2 + fc, b:b + 1], axis=0),
                      bounds_check=breg, oob_is_err=False), wd_, r=[idxw], w=[wd_])
            S.dma("sp", lambda e: e.dma_start(out=xb_[:], in_=Dd["XS"][b * MB:(b + 1) * MB, :].rearrange("(s p) d -> p s d", p=128)),
                  xb_, w=[xb_])
            yield
            for s_ in range(NS):
                for c in range(8):
                    S.op("pe", lambda e, c=c, s_=s_: e.transpose(prb[:, c, :], xb_[:, s_, c * 128:(c + 1) * 128], identb[:]),
                         r=[xb_, identb], w=[pr])
                if s_ % 2 == 0:
                    S.op("act", lambda e, s_=s_: e.copy(xT[:, :, s_ * 128:(s_ + 1) * 128], prb), r=[pr], w=[xT])
                else:
                    S.op("dve", lambda e, s_=s_: e.tensor_copy(xT[:, :, s_ * 128:(s_ + 1) * 128], prb), r=[pr], w=[xT])
                yield
            for fc in range(4):
                sg_ = sg[fc % 2]
                pg_, pu_ = (bk[1], bk[2]) if fc % 2 == 0 else (bk[3], bk[0])
                for j in range(8):
                    S.op("pe", _mm(pg_[:, 0:MB], wg_[:, j, fc * 128:(fc + 1) * 128], xT[:, j, :], j == 0, j == 7), r=[wg_, xT], w=[pg_])
                for j in range(8):
                    S.op("pe", _mm(pu_[:, 0:MB], wu_[:, j, fc * 128:(fc + 1) * 128], xT[:, j, :], j == 0, j == 7), r=[wu_, xT], w=[pu_])
                S.op("act", lambda e, sg_=sg_, pg_=pg_: e.activation(out=sg_[:], in_=pg_[:, 0:MB], func=AF.Silu), r=[pg_], w=[sg_])
                S.op("dve", lambda e, sg_=sg_, fc=fc, pu_=pu_: e.tensor_tensor(hT[:, fc, :], pu_[:, 0:MB], sg_[:], ALU.mult), r=[pu_, sg_], w=[hT])
                yield
            for s_ in range(NS):
                y_ = yt[nyt % 2]
                nyt += 1
                for h in range(2):
                    py_ = bk[(2 * s_ + h + 1) % 4]
                    for fc in range(4):
                        S.op("pe", _mm(py_[:], hT[:, fc, s_ * 128:(s_ + 1) * 128], wd_[:, fc, h * 512:(h + 1) * 512], fc == 0, fc == 3),
                             r=[hT, wd_], w=[py_])
                    if h == 0:
                        S.op("act", lambda e, y_=y_, py_=py_: e.copy(y_[:, 0:512], py_[:]), r=[py_], w=[y_])
                    else:
                        S.op("dve", lambda e, y_=y_, py_=py_: e.tensor_copy(y_[:, 512:1024], py_[:]), r=[py_], w=[y_])
                r0 = b * MB + s_ * 128
                S.dma("sp", lambda e, y_=y_, r0=r0: e.dma_start(out=Dd["Y"][r0:r0 + 128, :], in_=y_[:]), y_, r=[y_])
                yield

    gens = [streamD(0, list(range(0, NBLK // 2))), streamD(1, list(range(NBLK // 2, NBLK)))]
    for _ in range(D_OFFSET):
        next(gens[0])
    run_streams(gens)


def phase_E(K, pes, P):
    nc, S, I, Dd = K.nc, K.S, K.I, K.Dd
    NT = K.NT
    sb, ps = mk_alloc(K, pes)
    gw, d1i, neghalf, identb = P["gw"], P["d1i"], P["neghalf"], P["identb"]
    gple = load_colvec(K, sb, "gple", I["norm_ple_g"], 8)
    wst = [sb(f"wstE{i}", [128, 2048], F32, dma=True) for i in range(2)]
    wpg = load_weight_bf(K, sb, "wpg", I["w_ple_gate"], D, D, gvec=gple, stage=wst)
    wpp = load_weight_bf(K, sb, "wpp", I["w_ple_proj"], 256, D)
    gfin = load_rowbc(K, sb, "gfin", I["final_norm_g"], D)
    junk = sb("junkE", [128, D], BF16)

    def streamE(sid, tiles):
        def T(n, shape, dt, dma=False):
            return sb(f"{n}E{sid}", shape, dt, dma=dma)
        hL = [T(f"h1t{i}", [128, D], F32, True) for i in range(2)]
        yaL = [T(f"ya{i}", [128, D], BF16, "sw") for i in range(2)]
        ybL = [T(f"yb{i}", [128, D], BF16, "sw") for i in range(2)]
        pL_ = [T(f"pt{i}", [128, 256], F32, True) for i in range(2)]
        o_ = T("ot", [128, D], F32, True)
        h2 = T("h2", [128, D], F32)
        ss = T("ss", [128, 1], F32)
        vv = T("vv", [128, 1], F32)
        rstd = T("rstd", [128, 1], F32)
        u3 = T("u3", [128, D], BF16)
        u3T = T("u3T", [128, 8, 128], BF16)
        pb_ = T("pb", [128, 256], BF16)
        pT_ = T("pT", [128, 2, 128], BF16)
        pgt = T("pgt", [128, D], F32)
        pA_ = ps(f"pzE{sid}", [128, 512])
        pr = ps(f"prE{sid}", [128, 512])
        pz = [pA_, pA_]
        pq = pr
        prb = pr.t[:].bitcast(BF16).rearrange("p (c t) -> p c t", c=8)
        def loads(i):
            t = tiles[i]
            h_, ya_, yb_, p_ = hL[i % 2], yaL[i % 2], ybL[i % 2], pL_[i % 2]
            S.dma("sp", lambda e: e.dma_start(out=h_[:], in_=Dd["H1"][t * 128:(t + 1) * 128, :]), h_, w=[h_])
            S.dma("sp", lambda e: e.dma_start(out=p_[:], in_=I["p"][t * 128:(t + 1) * 128, :]), p_, w=[p_])
            for kk, y_ in ((0, ya_), (1, yb_)):
                S.dma("pool", lambda e, y_=y_, kk=kk: e.indirect_dma_start(out=y_[:], out_offset=None, in_=Dd["Y"],
                      in_offset=bass.IndirectOffsetOnAxis(ap=d1i[:, kk, t:t + 1], axis=0)), y_, r=[d1i], w=[y_])

        loads(0)
        for i, t in enumerate(tiles):
            h_, ya_, yb_, p_ = hL[i % 2], yaL[i % 2], ybL[i % 2], pL_[i % 2]
            if i + 1 < len(tiles):
                loads(i + 1)
            yield
            S.op("dve", lambda e: e.scalar_tensor_tensor(h2[:], ya_[:], gw[:, 0, t:t + 1], h_[:], ALU.mult, ALU.add),
                 r=[ya_, gw, h_], w=[h2])
            S.op("dve", lambda e: e.scalar_tensor_tensor(h2[:], yb_[:], gw[:, 1, t:t + 1], h2[:], ALU.mult, ALU.add),
                 r=[yb_, gw, h2], w=[h2])
            S.op("act", lambda e: e.activation(out=junk[:], in_=h2[:], func=AF.Square, accum_out=ss[:, 0:1]), r=[h2], w=[junk, ss])
            rstd_from_ss(K, ss, rstd, vv, neghalf)
            yield
            S.op("act", lambda e: e.activation(out=u3[:], in_=h2[:], func=AF.Copy, scale=rstd[:, 0:1]), r=[h2, rstd], w=[u3])
            for c in range(8):
                S.op("pe", lambda e, c=c: e.transpose(prb[:, c, :], u3[:, c * 128:(c + 1) * 128], identb[:]), r=[u3, identb], w=[pr])
            S.op("act", lambda e: e.copy(u3T[:], prb), r=[pr], w=[u3T])
            yield
            S.op("dve", lambda e: e.tensor_copy(pb_[:], p_[:]), r=[p_], w=[pb_])
            for c in range(2):
                S.op("pe", lambda e, c=c: e.transpose(prb[:, c, :], pb_[:, c * 128:(c + 1) * 128], identb[:]), r=[pb_, identb], w=[pr])
            S.op("act", lambda e: e.copy(pT_[:], prb[:, 0:2, :]), r=[pr], w=[pT_])
            yield
            for h in range(2):
                sl = slice(h * 512, (h + 1) * 512)
                for c in range(8):
                    S.op("pe", _mm(pz[h][:], u3T[:, c, :], wpg[:, c, sl], c == 0, c == 7), r=[u3T, wpg], w=[pz[h]])
                S.op("act", lambda e, h=h, sl=sl: e.activation(out=pgt[:, sl], in_=pz[h][:], func=AF.Sigmoid), r=[pz[h]], w=[pgt])
                for c in range(2):
                    S.op("pe", _mm(pq[:], pT_[:, c, :], wpp[:, c, sl], c == 0, c == 1), r=[pT_, wpp], w=[pq])
                S.op("dve", lambda e, h=h, sl=sl: e.tensor_tensor(pgt[:, sl], pq[:], pgt[:, sl], ALU.mult), r=[pq, pgt], w=[pgt])
                yield
            S.op("dve", lambda e: e.tensor_tensor(h2[:], h2[:], pgt[:], ALU.add), r=[h2, pgt], w=[h2])
            S.op("act", lambda e: e.activation(out=junk[:], in_=h2[:], func=AF.Square, accum_out=ss[:, 0:1]), r=[h2], w=[junk, ss])
            rstd_from_ss(K, ss, rstd, vv, neghalf)
            yield
            S.op("dve", lambda e: e.scalar_tensor_tensor(o_[:], h2[:], rstd[:, 0:1], gfin[:], ALU.mult, ALU.mult),
                 r=[h2, rstd, gfin], w=[o_])
            S.dma("sp", lambda e: e.dma_start(out=K.out[t * 128:(t + 1) * 128, :], in_=o_[:]), o_, r=[o_])
            yield

    gens = [streamE(i, list(range(i, NT, 4))) for i in range(4)]
    for i in range(3):
        for _ in range(2 * (3 - i)):
            next(gens[i])
    run_streams(gens)
    S.barrier()
```
